# Optimizing a Trainium2 kernel written in Bass

```python
import math
import jax, jax.numpy as jnp
from jax import lax
import numpy as np

D_MODEL = 1024
BATCH = 8
SEQ = 2048
DEPTH = 2

GRID_W = 64
CTX_LEN = 256

D_MIX = D_MODEL
D_FOURIER = D_MIX // 4
FOURIER_GROUPS = 4
D_FG = D_FOURIER // FOURIER_GROUPS
HEAD_DIM = 64
NA_HEADS = (D_MIX // 2) // HEAD_DIM
D_NA = NA_HEADS * HEAD_DIM
D_CONV = D_MIX - D_FOURIER - D_NA
CONV_WIDTH = 3
D_IN_PROJ = D_FOURIER + 3 * D_NA + 3 * D_CONV

NA_KH = 8
NA_KW = 16
NA_QB = 16
NA_KB = 32

N_GROUPS = 4
EXPERTS_PER_GROUP = 8
N_EXPERTS = N_GROUPS * EXPERTS_PER_GROUP
TOP_K = 2
D_EXPERT = 512

EPS = 1e-6
NEG_INF = -1e30

kernel_name = "hybrid_fourier_natten_shortconv_hmoe_dit"


def rms_norm(x, gain):
    xf = x.astype(jnp.float32)
    y = xf * lax.rsqrt(jnp.mean(xf * xf, axis=-1, keepdims=True) + EPS)
    return (y * gain.astype(jnp.float32)).astype(x.dtype)


def split_in(u):
    o = [D_FOURIER, D_FOURIER + D_NA, D_FOURIER + 2 * D_NA, D_FOURIER + 3 * D_NA,
         D_FOURIER + 3 * D_NA + D_CONV, D_FOURIER + 3 * D_NA + 2 * D_CONV]
    f, q, k, v, gb, gc, h = jnp.split(u, o, axis=-1)
    heads = lambda t: t.reshape(*t.shape[:-1], NA_HEADS, HEAD_DIM)
    return f, heads(q), heads(k), heads(v), gb, gc, h


def fourier_mix(f):
    b, n, _ = f.shape
    fg = f.reshape(b, n, FOURIER_GROUPS, D_FG).astype(jnp.float32)
    out = jnp.fft.fft2(fg, axes=(1, 3), norm="ortho").real
    return out.reshape(b, n, D_FOURIER).astype(f.dtype)


def short_conv(h, w):
    return lax.conv_general_dilated(
        h, w[:, None, :].astype(h.dtype), window_strides=(1,), padding=((1, 1),),
        dimension_numbers=("NWC", "WIO", "NWC"), feature_group_count=h.shape[-1])


def na_latent(q, k, v, kc, vc, rpb):
    b, n, h, dh = q.shape
    rows = n // GRID_W
    kh = min(NA_KH, rows)
    ncb = GRID_W // NA_QB
    r = jnp.arange(rows)
    row_start = jnp.clip(r - kh // 2, 0, rows - kh)
    row_idx = row_start[:, None] + jnp.arange(kh)[None, :]
    cols = jnp.arange(GRID_W)
    col_start = jnp.clip(cols - NA_KW // 2, 0, GRID_W - NA_KW)
    band_start = jnp.clip(col_start[::NA_QB], 0, GRID_W - NA_KB)
    band_idx = band_start[:, None] + jnp.arange(NA_KB)[None, :]
    qcol = cols.reshape(ncb, NA_QB)
    qcs = col_start.reshape(ncb, NA_QB)
    kcol = band_idx[:, None, :]
    valid = (kcol >= qcs[..., None]) & (kcol < qcs[..., None] + NA_KW)
    roff = row_idx - r[:, None] + (NA_KH - 1)
    coff = jnp.clip(kcol - qcol[..., None] + (NA_KW - 1), 0, 2 * NA_KW - 2)
    bias = rpb[:, roff[:, None, None, :, None], coff[None, :, :, None, :]]

    qg = q.reshape(b, rows, ncb, NA_QB, h, dh)
    kg = k.reshape(b, rows, GRID_W, h, dh)
    vg = v.reshape(b, rows, GRID_W, h, dh)
    ri = row_idx[:, :, None, None]
    ci = band_idx[None, None]
    kb = kg[:, ri, ci]
    vb = vg[:, ri, ci]

    s_loc = jnp.einsum("brcqhd,brkcjhd->bhrcqkj", qg, kb).astype(jnp.float32)
    s_loc = jnp.where(valid[:, :, None, :], s_loc + bias.astype(jnp.float32), NEG_INF)
    s_ctx = jnp.einsum("brcqhd,blhd->bhrcql", qg, kc).astype(jnp.float32)
    kj = kh * NA_KB
    s = jnp.concatenate([s_loc.reshape(*s_loc.shape[:5], kj), s_ctx], axis=-1)
    p = jax.nn.softmax(s, axis=-1).astype(v.dtype)
    p_loc = p[..., :kj].reshape(s_loc.shape)
    p_ctx = p[..., kj:]
    o = (jnp.einsum("bhrcqkj,brkcjhd->brcqhd", p_loc, vb)
         + jnp.einsum("bhrcql,blhd->brcqhd", p_ctx, vc))
    return o.reshape(b, n, h * dh)


def na_context(qc, kc, vc):
    s = jnp.einsum("blhd,bmhd->bhlm", qc, kc).astype(jnp.float32)
    p = jax.nn.softmax(s, axis=-1).astype(vc.dtype)
    o = jnp.einsum("bhlm,bmhd->blhd", p, vc)
    return o.reshape(*o.shape[:2], D_NA)


def mix_out(f, attn, conv, w_fourier, w_out):
    return jnp.concatenate([fourier_mix(f) @ w_fourier, attn, conv], axis=-1) @ w_out


def hier_moe(t, w_rg, b_rg, w_re, b_re, w_gate, w_up, w_down):
    n_tok = t.shape[0]
    g_logits = (t @ w_rg + b_rg).astype(jnp.float32)
    g_prob = jax.nn.softmax(g_logits, axis=-1)
    g_idx = jnp.argmax(g_logits, axis=-1)
    g_w = jnp.take_along_axis(g_prob, g_idx[:, None], axis=-1)
    e_logits = (t @ w_re + b_re).astype(jnp.float32).reshape(n_tok, N_GROUPS, EXPERTS_PER_GROUP)
    e_sel = jnp.take_along_axis(e_logits, g_idx[:, None, None], axis=1)[:, 0]
    top_v, top_i = lax.top_k(e_sel, TOP_K)
    pair_w = g_w * jax.nn.softmax(top_v, axis=-1)
    expert_id = g_idx[:, None] * EXPERTS_PER_GROUP + top_i
    combine = jnp.sum(jax.nn.one_hot(expert_id, N_EXPERTS, dtype=jnp.float32)
                      * pair_w[..., None], axis=1).astype(t.dtype)
    y = jnp.zeros_like(t)
    for e in range(N_EXPERTS):
        he = jax.nn.silu(t @ w_gate[e]) * (t @ w_up[e])
        y = y + combine[:, e:e + 1] * (he @ w_down[e])
    return y


def trunk_layer(x, xc, mod_x, mod_c, norm1, norm2, w_in, w_fourier, w_conv, rpb, w_out,
                w_rg, b_rg, w_re, b_re, w_gate, w_up, w_down, last):
    b, n, d = x.shape
    l = xc.shape[1]
    sh1, sc1, g1, sh2, sc2, g2 = jnp.split(mod_x[:, None, :], 6, axis=-1)
    csh1, csc1, cg1, csh2, csc2, cg2 = jnp.split(mod_c, 6, axis=-1)
    scale = 1.0 / math.sqrt(HEAD_DIM)

    ux = (rms_norm(x, norm1) * (1 + sc1) + sh1) @ w_in
    uc = (rms_norm(xc, norm1) * (1 + csc1) + csh1) @ w_in
    fx, qx, kx, vx, bx, cx, hx = split_in(ux)
    fc, qc, kc, vc, bc, cc, hc = split_in(uc)
    attn_x = na_latent(qx * scale, kx, vx, kc, vc, rpb)
    conv_x = bx * short_conv(cx * hx, w_conv)
    x = x + g1 * mix_out(fx, attn_x, conv_x, w_fourier, w_out)
    if not last:
        attn_c = na_context(qc * scale, kc, vc)
        conv_c = bc * short_conv(cc * hc, w_conv)
        xc = xc + cg1 * mix_out(fc, attn_c, conv_c, w_fourier, w_out)

    hx2 = rms_norm(x, norm2) * (1 + sc2) + sh2
    if last:
        y = hier_moe(hx2.reshape(-1, d), w_rg, b_rg, w_re, b_re, w_gate, w_up, w_down)
        x = x + g2 * y.reshape(b, n, d)
    else:
        hc2 = rms_norm(xc, norm2) * (1 + csc2) + csh2
        tok = jnp.concatenate([hx2.reshape(-1, d), hc2.reshape(-1, d)], axis=0)
        y = hier_moe(tok, w_rg, b_rg, w_re, b_re, w_gate, w_up, w_down)
        x = x + g2 * y[:b * n].reshape(b, n, d)
        xc = xc + cg2 * y[b * n:].reshape(b, l, d)
    return x, xc


def setup_inputs(seed: int = 0) -> dict:
    key = jax.random.key(seed)
    ks = jax.random.split(key, 24)
    d = D_MODEL
    nrm = lambda k, shape, s: jax.random.normal(k, shape, jnp.float32) * s
    return {
        "x": nrm(ks[0], (BATCH, SEQ, d), 1.0),
        "c": nrm(ks[1], (BATCH, d), 1.0),
        "ctx": nrm(ks[2], (BATCH, CTX_LEN, d), 1.0),
        "c_ctx": nrm(ks[3], (d,), 1.0),
        "w_ada": nrm(ks[4], (DEPTH, d, 6 * d), 0.5 * d ** -0.5),
        "b_ada": nrm(ks[5], (DEPTH, 6 * d), 0.02),
        "norm1": 1.0 + nrm(ks[6], (DEPTH, d), 0.05),
        "norm2": 1.0 + nrm(ks[7], (DEPTH, d), 0.05),
        "w_in": nrm(ks[8], (DEPTH, d, D_IN_PROJ), d ** -0.5),
        "w_fourier": nrm(ks[9], (DEPTH, D_FOURIER, D_FOURIER), D_FOURIER ** -0.5),
        "w_conv": nrm(ks[10], (DEPTH, CONV_WIDTH, D_CONV), CONV_WIDTH ** -0.5),
        "rpb": nrm(ks[11], (DEPTH, NA_HEADS, 2 * NA_KH - 1, 2 * NA_KW - 1), 0.1),
        "w_out": nrm(ks[12], (DEPTH, D_MIX, d), D_MIX ** -0.5),
        "w_rg": nrm(ks[13], (DEPTH, d, N_GROUPS), d ** -0.5),
        "b_rg": nrm(ks[14], (DEPTH, N_GROUPS), 0.01),
        "w_re": nrm(ks[15], (DEPTH, d, N_EXPERTS), d ** -0.5),
        "b_re": nrm(ks[16], (DEPTH, N_EXPERTS), 0.01),
        "w_gate": nrm(ks[17], (DEPTH, N_EXPERTS, d, D_EXPERT), d ** -0.5),
        "w_up": nrm(ks[18], (DEPTH, N_EXPERTS, d, D_EXPERT), d ** -0.5),
        "w_down": nrm(ks[19], (DEPTH, N_EXPERTS, D_EXPERT, d), D_EXPERT ** -0.5),
        "norm_final": 1.0 + nrm(ks[20], (d,), 0.05),
    }


def reference(x, c, ctx, c_ctx, w_ada, b_ada, norm1, norm2, w_in, w_fourier, w_conv, rpb,
              w_out, w_rg, b_rg, w_re, b_re, w_gate, w_up, w_down, norm_final):
    xc = ctx
    sc = jax.nn.silu(c)
    scc = jax.nn.silu(c_ctx)
    for li in range(DEPTH):
        mod_x = sc @ w_ada[li] + b_ada[li]
        mod_c = scc @ w_ada[li] + b_ada[li]
        x, xc = trunk_layer(x, xc, mod_x, mod_c, norm1[li], norm2[li], w_in[li], w_fourier[li],
                            w_conv[li], rpb[li], w_out[li], w_rg[li], b_rg[li], w_re[li],
                            b_re[li], w_gate[li], w_up[li], w_down[li], li == DEPTH - 1)
    return rms_norm(x, norm_final)
```

```python
import numpy as np
import ml_dtypes
from contextlib import ExitStack
import concourse.bass as bass
import concourse.mybir as mybir
from concourse.bass_utils import run_bass_kernel_spmd

F32 = mybir.dt.float32
BF16 = mybir.dt.bfloat16
U8 = mybir.dt.uint8
I32 = mybir.dt.int32
AF = mybir.ActivationFunctionType
ALU = mybir.AluOpType
AX = mybir.AxisListType

COMPUTE = ("pe", "act", "dve", "pool")
import os
SAME_ENGINE_SYNC = os.environ.get('SES', '1') == '1'
ATT_SKEW = os.environ.get('ATT_SKEW', '0') == '1'


class Reg:
    __slots__ = ("writer", "rd", "rdma")

    def __init__(self):
        self.writer = None
        self.rd = {}
        self.rdma = []


def _inherit(g, parents):
    for p in parents:
        cands = list(p.rd.values())
        if p.writer is not None:
            cands.append(p.writer)
        for op in cands:
            if op.dma:
                g.rdma.append(op)
            else:
                o = g.rd.get(op.eng)
                if o is None or o.pos < op.pos:
                    g.rd[op.eng] = op
        g.rdma.extend(p.rdma)


class Buf:
    def __init__(self, ap, parents=()):
        self.ap = ap
        self.parents = list(parents)
        self.regs = {}

    def r(self, key=0):
        g = self.regs.get(key)
        if g is None:
            g = Reg()
            for pb in self.parents:
                _inherit(g, pb.allregs())
            self.regs[key] = g
        return g

    def rs(self, keys):
        return [self.r(k) for k in keys]

    def allregs(self):
        return list(self.regs.values())


class Op:
    __slots__ = ("eng", "fn", "deps", "dma", "pos", "waits", "signal", "dsem", "dval", "dprev")


class Prog:
    def __init__(self, nc, n_dma_sems=20):
        self.nc = nc
        self.ops = []
        self.streams = {e: [] for e in ("pe", "act", "dve", "pool", "sp")}
        self.n_dma_sems = n_dma_sems
        self.dma_rr = {"sp": 0, "pool": 0, "act": 0}
        self.dma_tot = {}
        self.final_dmas = []

    def add(self, eng, fn, reads=(), writes=(), dma=False):
        op = Op()
        op.eng = eng
        op.fn = fn
        op.dma = dma
        op.waits = []
        op.signal = False
        deps = []
        for r in reads:
            if r.writer is not None:
                deps.append(r.writer)
        for w in writes:
            if w.writer is not None:
                deps.append(w.writer)
            deps.extend(w.rd.values())
            deps.extend(w.rdma)
        for r in reads:
            if dma:
                r.rdma.append(op)
            else:
                r.rd[eng] = op
        for w in writes:
            w.writer = op
            w.rd = {}
            w.rdma = []
        op.deps = deps
        op.pos = len(self.streams[eng])
        self.streams[eng].append(op)
        self.ops.append(op)
        if dma:
            k = self.dma_rr[eng]
            self.dma_rr[eng] = (k + 1) % self.n_dma_sems
            key = (eng, k)
            prev = self.dma_tot.get(key, 0)
            op.dsem = key
            op.dprev = prev
            op.dval = prev + 16
            self.dma_tot[key] = prev + 16
        return op

    def emit(self, stack):
        nc = self.nc
        sems = {e: stack.enter_context(nc.semaphore("s_" + e)) for e in COMPUTE}
        dsems = {}
        for key in self.dma_tot:
            dsems[key] = stack.enter_context(nc.semaphore("d_%s%d" % key))
        waited = {c: {} for c in self.streams}
        for op in self.ops:
            c = op.eng
            w = waited[c]
            best = {}
            for d in op.deps:
                if d is op:
                    continue
                if d.dma:
                    if w.get(d.dsem, 0) < d.dval:
                        w[d.dsem] = d.dval
                        op.waits.append(("d", d.dsem, d.dval))
                else:
                    if d.eng == c and (c == "pe" or not SAME_ENGINE_SYNC):
                        continue
                    if d.pos > best.get(d.eng, -1):
                        best[d.eng] = d.pos
            for e, p in best.items():
                if w.get(e, -1) < p:
                    w[e] = p
                    prod = self.streams[e][p]
                    prod.signal = True
                    op.waits.append(("c", e, prod))
            if op.dma and op.dprev > 0:
                if w.get(op.dsem, 0) < op.dprev:
                    w[op.dsem] = op.dprev
                    op.waits.append(("d", op.dsem, op.dprev))
            op.deps = None
        for e in COMPUTE:
            cnt = 0
            for op in self.streams[e]:
                if op.signal and not op.dma:
                    cnt += 1
                    op.dval = cnt
        final = list(self.final_dmas)

        def run_stream(engname, eng):
            for op in self.streams[engname]:
                for kind, key, v in op.waits:
                    if kind == "d":
                        eng.wait_ge(dsems[key], v)
                    else:
                        eng.wait_ge(sems[key], v.dval)
                ins = op.fn(eng)
                if op.dma:
                    ins.then_inc(dsems[op.dsem], 16)
                elif op.signal:
                    ins.then_inc(sems[engname], 1)
            if engname == "sp":
                for op in final:
                    eng.wait_ge(dsems[op.dsem], op.dval)

        with nc.Block() as block:
            @block.sync
            def _(e):
                run_stream("sp", e)

            @block.tensor
            def _(e):
                run_stream("pe", e)

            @block.scalar
            def _(e):
                run_stream("act", e)

            @block.vector
            def _(e):
                run_stream("dve", e)

            @block.gpsimd
            def _(e):
                run_stream("pool", e)


def seq(fns):
    def f(e):
        ins = None
        for g in fns:
            ins = g(e)
        return ins
    return f


D = 1024
NX = 2048
NC_ = 256
NT = NX + NC_
NE = 32
EPS = 1e-6
TILES = [(0, 512), (512, 512), (1024, 512), (1536, 512), (2048, 256)]
SCR_BYTES = 97400
NTILES = (40, 39)
NSLOT = 40 * 512


def cs_of(j):
    return min(max(j - 2, 0), 11)


def cls_of(j):
    return {0: 0, 1: 1, 14: 3, 15: 4}.get(j, 2)


def MM(out, lhsT, rhs, start=True, stop=True):
    return lambda e: e.matmul(out, lhsT, rhs, start=start, stop=stop)


def TR(out, in_, idn):
    return lambda e: e.transpose(out, in_, idn)


def ACT(out, in_, func, **kw):
    return lambda e: e.activation(out=out, in_=in_, func=func, **kw)


def TT(out, in0, in1, op):
    return lambda e: e.tensor_tensor(out=out, in0=in0, in1=in1, op=op)


def STT(out, in0, scalar, in1, op0, op1):
    return lambda e: e.scalar_tensor_tensor(out=out, in0=in0, scalar=scalar, in1=in1, op0=op0, op1=op1)


def TS(out, in0, s1, op0):
    return lambda e: e.tensor_scalar(out=out, in0=in0, scalar1=s1, scalar2=None, op0=op0)


def RCP(out, in_):
    return lambda e: e.reciprocal(out=out, in_=in_)


def RED(out, in_, op):
    return lambda e: e.tensor_reduce(out=out, in_=in_, axis=AX.X, op=op)


def MEMSET(ap, v):
    return lambda e: e.memset(ap, v)


def build_program(n_layers=2, taps=(), moe_experts=NE, do_mixer=True):
    nc = bass.Bass("TRN2", target_bir_lowering=False)
    st = ExitStack()
    pg = Prog(nc)
    tapset = set(taps)
    tap_out = {}

    def dram(name, shape, dt=F32, out=False):
        return nc.dram_tensor(name, list(shape), dt, kind="ExternalOutput" if out else "ExternalInput").ap()

    xin = dram("xin", [NT, D])
    cvec_d = dram("cvec", [128, 16])
    w_ada = dram("w_ada", [2, D, 6 * D])
    bada_d = dram("bada", [128, 96])
    nrm_d = dram("nrm", [128, 40])
    w_in = dram("w_in", [2, D, 2560])
    w_out = dram("w_out", [2, D, D])
    wf_d = dram("wf", [2, 256, 256])
    wconv_d = dram("wconv", [128, 12])
    wr_d = dram("wr", [2, D, 36])
    br_d = dram("br", [128, 72])
    W_gu = dram("W_gu", [2 * NE * 128, 8192])
    W_dn = dram("W_dn", [2 * NE * 128, 4096])
    U_d = dram("Uc", [128, 128])
    tau_d = dram("tau", [128, 40])
    pconst_d = dram("pconst", [128, 2])
    Hs_ap = nc.dram_tensor("Hs", [NSLOT, D], BF16, kind="Internal").ap()
    R_ap = nc.dram_tensor("Rr", [NSLOT, D], F32, kind="Internal").ap()
    biasT_d = dram("biasT", [2, 8, 128, 3200])
    CN_d = dram("CN", [NX, NX], BF16)
    SN_d = dram("SN", [NX, NX], BF16)
    C2_d = dram("C2", [256, 256], BF16)
    S2_d = dram("S2", [256, 256], BF16)
    BCS_d = dram("BCS", [256, 512])
    ident_d = dram("ident", [128, 128])
    out_d = dram("out", [NX, D], out=True)

    def sbt(name, shape, dt=F32):
        return st.enter_context(nc.sbuf_tensor(name, list(shape), dt))

    xT = Buf(sbt("xT", [128, 8, NT])[:])
    hT_t = sbt("hT", [128, 8 * NT], BF16)
    hT = Buf(hT_t[:].rearrange("p (c t) -> p c t", c=8))
    ident = Buf(sbt("ident_sb", [128, 128])[:])
    identb = Buf(sbt("identb", [128, 128], BF16)[:])
    onesb = Buf(sbt("onesb", [128, 128], BF16)[:])
    cv = Buf(sbt("cv", [128, 16])[:])
    scv = Buf(sbt("scv", [128, 16])[:])
    bada = Buf(sbt("bada_sb", [128, 96])[:])
    nrm = Buf(sbt("nrm_sb", [128, 40])[:])
    wconv = Buf(sbt("wconv_sb", [128, 12])[:])
    brs = Buf(sbt("br_sb", [128, 72])[:])
    mod = Buf(sbt("mod", [128, 2, 48, 2])[:])
    gm = Buf(sbt("gm", [128, 2, 2, 8, 2])[:])
    Ub = Buf(sbt("Ub", [128, 128], BF16)[:])
    tau = Buf(sbt("tau_sb", [128, 40])[:])
    pconst = Buf(sbt("pconst_sb", [128, 2])[:])
    pw1 = [Buf(sbt("pw1_%d" % l_, [128, 18])[:]) for l_ in range(2)]
    pw2 = [Buf(sbt("pw2_%d" % l_, [128, 18])[:]) for l_ in range(2)]
    posi = [[Buf(sbt("posi%d_%d" % (l_, k_), [128, 18], I32)[:]) for k_ in range(2)] for l_ in range(2)]
    widx = [Buf(sbt("widx%d" % l_, [128, 40], I32)[:]) for l_ in range(2)]
    scr_t = sbt("scr", [128, SCR_BYTES], U8)
    ps_t = st.enter_context(nc.psum_tensor("ps", [128, 8, 512], F32))
    PS = Buf(ps_t[:])
    ps = PS.ap

    allocs = []
    cur = [0]

    def reset_scratch():
        cur[0] = 0

    def carve(shape, dt, at=None):
        esz = 4 if dt == F32 else 2
        n = 1
        for s_ in shape[1:]:
            n *= s_
        nbytes = (n * esz + 31) // 32 * 32
        off = cur[0] if at is None else at
        if at is None:
            cur[0] = off + nbytes
        assert off + nbytes <= SCR_BYTES, ("scratch overflow", off + nbytes)
        ap = scr_t[:, off:off + n * esz].bitcast(dt)
        if len(shape) == 3:
            ap = ap.rearrange("p (a b) -> p a b", a=shape[1])
        elif len(shape) == 4:
            ap = ap.rearrange("p (a b c) -> p a b c", a=shape[1], b=shape[2])
        parents = [b_ for (o, e_, b_) in allocs if o < off + nbytes and off < e_]
        b = Buf(ap, parents)
        b.off = off
        allocs.append((off, off + nbytes, b))
        return b

    bank_ctr = [0]
    nbanks = [8]

    def nb():
        b = bank_ctr[0] % nbanks[0]
        bank_ctr[0] += 1
        return b

    def npair():
        if bank_ctr[0] % 2:
            bank_ctr[0] += 1
        b = bank_ctr[0] % 8
        bank_ctr[0] += 2
        return b

    def PB(b):
        return PS.r(b)

    rr = {"cp": 0}

    def copy_op(out_ap, in_ap, reads, writes):
        rr["cp"] ^= 1
        if rr["cp"]:
            pg.add("act", lambda e: e.copy(out_ap, in_ap), reads=reads, writes=writes)
        else:
            pg.add("dve", lambda e: e.tensor_copy(out_ap, in_ap), reads=reads, writes=writes)

    def load(q, out_ap, in_ap, writes):
        return pg.add(q, lambda e: e.dma_start(out=out_ap, in_=in_ap), writes=writes, dma=True)

    def tap(name, ap, shape, dt, regs):
        if name not in tapset:
            return
        d = dram("tap_" + name, shape, dt, out=True)
        op = pg.add("sp", lambda e: e.dma_start(out=d, in_=ap), reads=regs, dma=True)
        pg.final_dmas.append(op)
        tap_out[name] = "tap_" + name

    def xregs0():
        return xT.rs([(0, t) for t in range(5)])

    load("sp", ident.ap, ident_d, [ident.r()])
    load("pool", identb.ap, ident_d, [identb.r()])
    load("sp", cv.ap, cvec_d, [cv.r()])
    load("sp", bada.ap, bada_d, [bada.r()])
    load("sp", nrm.ap, nrm_d, [nrm.r()])
    load("sp", wconv.ap, wconv_d, [wconv.r()])
    load("sp", brs.ap, br_d, [brs.r()])
    pg.add("dve", MEMSET(onesb.ap, 1.0), writes=[onesb.r()])
    load("pool", Ub.ap, U_d, [Ub.r()])
    load("sp", tau.ap, tau_d, [tau.r()])
    load("sp", pconst.ap, pconst_d, [pconst.r()])
    pg.add("act", ACT(scv.ap, cv.ap, AF.Silu), reads=[cv.r()], writes=[scv.r()])

    reset_scratch()
    xs = [carve([128, D], F32) for _ in range(2)]
    wa = [carve([128, 8, 512], BF16) for _ in range(2)]
    mrow = [carve([2, 512], F32) for _ in range(2)]
    for i in range(NT // 128):
        xb = xs[i % 2]
        load("sp", xb.ap, xin[i * 128:(i + 1) * 128, :], [xb.r()])
        t = i // 4
        for half in range(2):
            b = nb()
            fns = [TR(ps[:, b, c4 * 128:(c4 + 1) * 128], xb.ap[:, (half * 4 + c4) * 128:(half * 4 + c4 + 1) * 128], ident.ap) for c4 in range(4)]
            pg.add("pe", seq(fns), reads=[xb.r(), ident.r()], writes=[PB(b)])
            copy_op(xT.ap[:, half * 4:(half + 1) * 4, i * 128:(i + 1) * 128], ps[:, b, :].rearrange("p (a b) -> p a b", a=4),
                    [PB(b)], xT.rs([(c, t) for c in range(half * 4, half * 4 + 4)]))

    zt = carve([128, 8192], BF16)
    HsInit = Buf(Hs_ap)
    RInit = Buf(R_ap)
    HsPrev = [HsInit]
    RPrev = [RInit]
    pg.add("dve", MEMSET(zt.ap, 0.0), writes=[zt.r()])
    for r_ in range(0, NSLOT, 1024):
        pg.add("sp", lambda e, o=Hs_ap[r_:r_ + 1024, :].rearrange("(p a) n -> p (a n)", a=8): e.dma_start(out=o, in_=zt.ap),
               reads=[zt.r()], writes=[HsInit.r(r_)], dma=True)
    scvb = Buf(sbt("scvb", [128, 16], BF16)[:])
    pg.add("act", ACT(scvb.ap, cv.ap, AF.Silu), reads=[cv.r()], writes=[scvb.r()])

    def compute_mod(li, wab):
        for jb in range(12):
            wb = wab[jb % len(wab)]
            load("pool", wb.ap, w_ada[li, :, jb * 512:(jb + 1) * 512].rearrange("(kc p) n -> p kc n", p=128), [wb.r()])
            b = nb()
            fns = [MM(ps[0:2, b, :], scvb.ap[:, kc * 2:kc * 2 + 2], wb.ap[:, kc, :], start=(kc == 0), stop=(kc == 7)) for kc in range(8)]
            pg.add("pe", seq(fns), reads=[wb.r(), scvb.r()], writes=[PB(b)])
            mr = mrow[jb % 2]
            pg.add("act", lambda e, o=mr.ap[0:2, :], i_=ps[0:2, b, :]: e.copy(o, i_), reads=[PB(b)], writes=[mr.r()])
            b2 = nb()
            fns = [TR(ps[:, b2, j4 * 2:j4 * 2 + 2], mr.ap[0:2, j4 * 128:(j4 + 1) * 128], ident.ap[0:2, 0:2]) for j4 in range(4)]
            pg.add("pe", seq(fns), reads=[mr.r(), ident.r()], writes=[PB(b2)])
            pg.add("dve", TT(mod.ap[:, li, jb * 4:(jb + 1) * 4, :], ps[:, b2, 0:8].rearrange("p (a b) -> p a b", a=4),
                             bada.ap[:, li * 48 + jb * 4:li * 48 + jb * 4 + 4].unsqueeze(2).to_broadcast([128, 4, 2]), ALU.add),
                   reads=[PB(b2), bada.r()], writes=[mod.r(li)])
        for which in range(2):
            base = 8 if which == 0 else 32
            pg.add("dve", STT(gm.ap[:, li, which, :, :], mod.ap[:, li, base:base + 8, :], 1.0,
                              nrm.ap[:, li * 16 + which * 8:li * 16 + which * 8 + 8].unsqueeze(2).to_broadcast([128, 8, 2]),
                              ALU.add, ALU.mult), reads=[mod.r(li), nrm.r()], writes=[gm.r(li)])

    for li_ in range(n_layers):
        compute_mod(li_, wa)
    tap("mod", mod.ap.rearrange("p a b c -> p (a b c)"), [128, 192], F32, [mod.r(0)])
    tap("x0", xT.ap[:, 0, :], [128, NT], F32, xregs0())

    MSH1, MG1, MSH2, MG2 = 0, 16, 24, 40

    NBUF = {}

    def carve_norm():
        NBUF["sq"] = [carve([128, 8, 512], BF16) for _ in range(2)]
        NBUF["tmp"] = [carve([128, 512], F32) for _ in range(4)]
        NBUF["h32"] = [carve([128, 512], F32) for _ in range(4)]
        NBUF["rstd"] = [carve([128, 512], F32) for _ in range(2)]

    def rms_rstd(t, t0, tw):
        b = nb()
        sq = NBUF["sq"][t % 2]
        for hf in range(2):
            pg.add("act", ACT(sq.ap[:, hf * 4:(hf + 1) * 4, 0:tw], xT.ap[:, hf * 4:(hf + 1) * 4, t0:t0 + tw], AF.Square),
                   reads=xT.rs([(c, t) for c in range(hf * 4, hf * 4 + 4)]), writes=[sq.r(hf)])
        pg.add("pe", seq([MM(ps[:, b, 0:tw], onesb.ap, sq.ap[:, c, 0:tw], start=(c == 0), stop=(c == 7)) for c in range(8)]),
               reads=[sq.r(0), sq.r(1), onesb.r()], writes=[PB(b)])
        rs_ = NBUF["rstd"][t % 2]
        pg.add("act", ACT(rs_.ap[:, 0:tw], ps[:, b, 0:tw], AF.Sqrt, scale=1.0 / D, bias=EPS), reads=[PB(b)], writes=[rs_.r()])
        pg.add("dve", RCP(rs_.ap[:, 0:tw], rs_.ap[:, 0:tw]), reads=[rs_.r()], writes=[rs_.r()])
        return rs_

    def norm_phase(li, which, tiles, router=None):
        shb = MSH1 if which == 0 else MSH2
        for (t, (t0, tw)) in tiles:
            s = 0 if t < 4 else 1
            rs_ = rms_rstd(t, t0, tw)
            nch = tw // 128
            rb = [nb() for _ in range(nch)] if router is not None else []
            for c in range(8):
                tm = NBUF["tmp"][c % 4]
                pg.add("dve", STT(tm.ap[:, 0:tw], xT.ap[:, c, t0:t0 + tw], gm.ap[:, li, which, c, s:s + 1], rs_.ap[:, 0:tw], ALU.mult, ALU.mult),
                       reads=[xT.r((c, t)), gm.r(li), rs_.r()], writes=[tm.r()])
                sh_ap = mod.ap[:, li, shb + c, s:s + 1]
                if router is None:
                    pg.add("act", ACT(hT.ap[:, c, t0:t0 + tw], tm.ap[:, 0:tw], AF.Identity, bias=sh_ap, scale=1.0),
                           reads=[tm.r(), mod.r(li)], writes=[hT.r((c, t))])
                else:
                    wr_sb, lg = router
                    hh = NBUF["h32"][c % 4]
                    pg.add("dve", TS(hh.ap[:, 0:tw], tm.ap[:, 0:tw], sh_ap, ALU.add), reads=[tm.r(), mod.r(li)], writes=[hh.r()])
                    pg.add("act", lambda e, o=hT.ap[:, c, t0:t0 + tw], i_=hh.ap[:, 0:tw]: e.copy(o, i_), reads=[hh.r()], writes=[hT.r((c, t))])
                    for i in range(nch):
                        pg.add("pe", MM(ps[:, rb[i], 0:36], hh.ap[:, i * 128:(i + 1) * 128], wr_sb.ap[:, c, :], start=(c == 0), stop=(c == 7)),
                               reads=[hh.r(), wr_sb.r()], writes=[PB(rb[i])])
            if router is not None:
                wr_sb, lg = router
                for i in range(nch):
                    pg.add("dve", TT(lg.ap[:, t * 4 + i, :], ps[:, rb[i], 0:36], brs.ap[:, li * 36:(li + 1) * 36], ALU.add),
                           reads=[PB(rb[i]), brs.r()], writes=[lg.r()])

    def outproj_partial(li, wo, nk, src_fn, src_regs, tiles):
        for (t, (t0, tw)) in tiles:
            s = 0 if t < 4 else 1
            for j in range(8):
                b = nb()
                fns = [MM(ps[:, b, 0:tw], wo.ap[:, k, j * 128:(j + 1) * 128], src_fn(k, t0, tw), start=(k == 0), stop=(k == nk - 1)) for k in range(nk)]
                pg.add("pe", seq(fns), reads=[wo.r()] + src_regs(t), writes=[PB(b)])
                pg.add("dve", STT(xT.ap[:, j, t0:t0 + tw], ps[:, b, 0:tw], mod.ap[:, li, MG1 + j, s:s + 1], xT.ap[:, j, t0:t0 + tw], ALU.mult, ALU.add),
                       reads=[PB(b), mod.r(li), xT.r((j, t))], writes=[xT.r((j, t))])

    def inproj_T(wblk_fn, wreg, tiles, evac):
        for (t, (t0, tw)) in tiles:
            b = nb()
            fns = [MM(ps[:, b, 0:tw], wblk_fn(kc), hT.ap[:, kc, t0:t0 + tw], start=(kc == 0), stop=(kc == 7)) for kc in range(8)]
            pg.add("pe", seq(fns), reads=[wreg] + hT.rs([(kc, t) for kc in range(8)]), writes=[PB(b)])
            evac(t, t0, tw, b)

    for li in range(n_layers):
        last = (li == n_layers - 1)
        tiles_all = list(enumerate(TILES))
        tiles_x = tiles_all[:4]
        tiles_res = tiles_x if last else tiles_all
        nchunks_res = 16 if last else 18

        reset_scratch()
        carve_norm()
        norm_phase(li, 0, tiles_all)
        if li == 0:
            tap("h1", hT.ap[:, 0, :], [128, NT], BF16, hT.rs([(0, t) for t in range(5)]))

        if do_mixer:
            reset_scratch()
            wfb = carve([128, 8, 256], BF16)
            fT = carve([128, 2, NT], BF16)
            wf_sb = carve([128, 2, 256], F32)
            bcs_sb = carve([128, 2, 512], F32)
            WCS = carve([128, 2, 512], BF16)
            PQ = carve([128, 18, 512], BF16)
            tabs = [[carve([128, 16, 256], BF16) for _ in range(2)] for _ in range(2)]
            wo_f = carve([128, 2, D], BF16)
            c2 = carve([128, 2, 256], BF16)
            s2 = carve([128, 2, 256], BF16)
            FwT = carve([128, 2, NT], BF16, at=fT.off)
            load("pool", wfb.ap, w_in[li, :, 0:256].rearrange("(kc p) n -> p kc n", p=128), [wfb.r()])
            load("sp", wf_sb.ap, wf_d[li].rearrange("(kc p) n -> p kc n", p=128), [wf_sb.r()])
            load("sp", bcs_sb.ap, BCS_d.rearrange("(kc p) n -> p kc n", p=128), [bcs_sb.r()])
            load("pool", wo_f.ap, w_out[li, 0:256, :].rearrange("(kc p) n -> p kc n", p=128), [wo_f.r()])
            for mi in range(2):
                b = nb()
                fns = []
                for cs_ in range(2):
                    for kc in range(2):
                        fns.append(MM(ps[:, b, cs_ * 256:(cs_ + 1) * 256], bcs_sb.ap[:, kc, cs_ * 256 + mi * 128:cs_ * 256 + (mi + 1) * 128],
                                      wf_sb.ap[:, kc, :], start=(kc == 0), stop=(kc == 1)))
                pg.add("pe", seq(fns), reads=[bcs_sb.r(), wf_sb.r()], writes=[PB(b)])
                copy_op(WCS.ap[:, mi, :], ps[:, b, :], [PB(b)], [WCS.r()])
            for fc in range(2):
                def evac_f(t, t0, tw, b, fc=fc):
                    copy_op(fT.ap[:, fc, t0:t0 + tw], ps[:, b, 0:tw], [PB(b)], [fT.r((fc, t))])
                inproj_T((lambda fc: lambda kc: wfb.ap[:, kc, fc * 128:(fc + 1) * 128])(fc), wfb.r(), tiles_res, evac_f)
            for i in range(nchunks_res):
                b = nb()
                fns = [MM(ps[:, b, :], fT.ap[:, fc, i * 128:(i + 1) * 128], WCS.ap[:, fc, :], start=(fc == 0), stop=(fc == 1)) for fc in range(2)]
                pg.add("pe", seq(fns), reads=[WCS.r()] + fT.rs([(0, i // 4), (1, i // 4)]), writes=[PB(b)])
                copy_op(PQ.ap[:, i, :], ps[:, b, :], [PB(b)], [PQ.r(i)])
            for kt in range(8):
                tb = tabs[kt % 2]
                load("sp", tb[0].ap, CN_d[:, kt * 256:(kt + 1) * 256].rearrange("(nc p) k -> p nc k", p=128), [tb[0].r()])
                load("sp", tb[1].ap, SN_d[:, kt * 256:(kt + 1) * 256].rearrange("(nc p) k -> p nc k", p=128), [tb[1].r()])
                for fc in range(2):
                    b = nb()
                    fns = []
                    for n_ in range(16):
                        for cs_ in range(2):
                            fns.append(MM(ps[:, b, 0:256], PQ.ap[:, n_, cs_ * 256 + fc * 128:cs_ * 256 + (fc + 1) * 128], tb[cs_].ap[:, n_, :],
                                          start=(n_ == 0 and cs_ == 0), stop=(n_ == 15 and cs_ == 1)))
                    pg.add("pe", seq(fns), reads=[tb[0].r(), tb[1].r()] + PQ.rs(range(16)), writes=[PB(b)])
                    copy_op(FwT.ap[:, fc, kt * 256:(kt + 1) * 256], ps[:, b, 0:256], [PB(b)], [FwT.r(kt // 2)])
            if not last:
                load("sp", c2.ap, C2_d.rearrange("(nc p) k -> p nc k", p=128), [c2.r()])
                load("sp", s2.ap, S2_d.rearrange("(nc p) k -> p nc k", p=128), [s2.r()])
                for fc in range(2):
                    b = nb()
                    fns = []
                    for n_ in range(2):
                        for cs_, tb_ in ((0, c2), (1, s2)):
                            fns.append(MM(ps[:, b, 0:256], PQ.ap[:, 16 + n_, cs_ * 256 + fc * 128:cs_ * 256 + (fc + 1) * 128], tb_.ap[:, n_, :],
                                          start=(n_ == 0 and cs_ == 0), stop=(n_ == 1 and cs_ == 1)))
                    pg.add("pe", seq(fns), reads=[c2.r(), s2.r()] + PQ.rs([16, 17]), writes=[PB(b)])
                    copy_op(FwT.ap[:, fc, 2048:2304], ps[:, b, 0:256], [PB(b)], [FwT.r(4)])
            if li == 0:
                tap("FwT", FwT.ap[:, 0, :], [128, NT], BF16, FwT.rs(range(5)))
            outproj_partial(li, wo_f, 2, lambda k, t0, tw: FwT.ap[:, k, t0:t0 + tw], lambda t: [FwT.r(t)], tiles_res)
            if li == 0:
                tap("xF", xT.ap[:, 0, :], [128, NT], F32, xregs0())

            reset_scratch()
            wg = carve([128, 8, 768], BF16)
            zT = carve([128, 2, NT], BF16)
            gbT = carve([128, 2, NT], BF16)
            convT = carve([128, 2, NT], BF16)
            cacc = carve([128, NT], F32)
            gtmp = [carve([128, 512], F32) for _ in range(2)]
            wo_c = carve([128, 2, D], BF16)
            load("pool", wg.ap, w_in[li, :, 1792:2560].rearrange("(kc p) n -> p kc n", p=128), [wg.r()])
            load("pool", wo_c.ap, w_out[li, 768:1024, :].rearrange("(kc p) n -> p kc n", p=128), [wo_c.r()])
            for cc in range(2):
                def evac_gb(t, t0, tw, b, cc=cc):
                    copy_op(gbT.ap[:, cc, t0:t0 + tw], ps[:, b, 0:tw], [PB(b)], [gbT.r((cc, t))])
                inproj_T((lambda cc: lambda kc: wg.ap[:, kc, cc * 128:(cc + 1) * 128])(cc), wg.r(), tiles_res, evac_gb)

                def evac_gc(t, t0, tw, b, cc=cc):
                    g = gtmp[t % 2]
                    pg.add("act", lambda e, o=g.ap[:, 0:tw], i_=ps[:, b, 0:tw]: e.copy(o, i_), reads=[PB(b)], writes=[g.r()])
                    b2 = nb()
                    fns = [MM(ps[:, b2, 0:tw], wg.ap[:, kc, 512 + cc * 128:512 + (cc + 1) * 128], hT.ap[:, kc, t0:t0 + tw],
                              start=(kc == 0), stop=(kc == 7)) for kc in range(8)]
                    pg.add("pe", seq(fns), reads=[wg.r()] + hT.rs([(kc, t) for kc in range(8)]), writes=[PB(b2)])
                    pg.add("dve", TT(zT.ap[:, cc, t0:t0 + tw], g.ap[:, 0:tw], ps[:, b2, 0:tw], ALU.mult), reads=[g.r(), PB(b2)], writes=[zT.r((cc, t))])
                inproj_T((lambda cc: lambda kc: wg.ap[:, kc, 256 + cc * 128:256 + (cc + 1) * 128])(cc), wg.r(), tiles_res, evac_gc)
                segs = [(0, NX)] if last else [(0, NX), (NX, NT)]
                zregs = zT.rs([(cc, t) for (t, _) in tiles_res])
                wbase = li * 6 + cc * 3
                w0 = wconv.ap[:, wbase + 0:wbase + 1]
                w1 = wconv.ap[:, wbase + 1:wbase + 2]
                w2 = wconv.ap[:, wbase + 2:wbase + 3]
                for (a0, a1) in segs:
                    pg.add("dve", TS(cacc.ap[:, a0:a1], zT.ap[:, cc, a0:a1], w1, ALU.mult), reads=zregs + [wconv.r()], writes=[cacc.r()])
                    pg.add("dve", STT(cacc.ap[:, a0 + 1:a1], zT.ap[:, cc, a0:a1 - 1], w0, cacc.ap[:, a0 + 1:a1], ALU.mult, ALU.add),
                           reads=zregs + [wconv.r(), cacc.r()], writes=[cacc.r()])
                    pg.add("dve", STT(cacc.ap[:, a0:a1 - 1], zT.ap[:, cc, a0 + 1:a1], w2, cacc.ap[:, a0:a1 - 1], ALU.mult, ALU.add),
                           reads=zregs + [wconv.r(), cacc.r()], writes=[cacc.r()])
                    pg.add("dve", TT(convT.ap[:, cc, a0:a1], cacc.ap[:, a0:a1], gbT.ap[:, cc, a0:a1], ALU.mult),
                           reads=[cacc.r()] + gbT.rs([(cc, t) for (t, _) in tiles_res]), writes=[convT.r(cc)])
            if li == 0:
                tap("convT", convT.ap[:, 0, :], [128, NT], BF16, convT.rs([0, 1]))
            outproj_partial(li, wo_c, 2, lambda k, t0, tw: convT.ap[:, k, t0:t0 + tw], lambda t: convT.rs([0, 1]), tiles_res)
            if li == 0:
                tap("xC", xT.ap[:, 0, :], [128, NT], F32, xregs0())

            reset_scratch()
            qT = carve([128, NT], BF16)
            kTm = [carve([128, NT], BF16) for _ in range(2)]
            Va = carve([128, 18, 2, 65], BF16)
            attn = carve([128, 18, 512], BF16)
            biasb = carve([128, 2, 5, 640], BF16)
            tmpS = [carve([128, 640], F32) for _ in range(2)]
            PT = [carve([128, 7, 128], BF16) for _ in range(2)]
            attnT = carve([128, 4, 512], BF16)
            wo_a = carve([128, 4, D], BF16)
            wqkv = [carve([128, 3, 8, 128], BF16) for _ in range(2)]
            rec = [carve([128, 2], F32) for _ in range(2)]
            load("pool", wo_a.ap, w_out[li, 256:768, :].rearrange("(kc p) n -> p kc n", p=128), [wo_a.r()])
            pg.add("dve", MEMSET(Va.ap[:, :, :, 64:65], 1.0), writes=[Va.r("ones")])
            pg.add("dve", MEMSET(kTm[0].ap[64:128, :], 0.0), writes=[kTm[0].r("z")])
            pg.add("dve", MEMSET(kTm[1].ap[0:64, :], 0.0), writes=[kTm[1].r("z")])
            nqb = 16 if last else 18
            for hp in range(4):
                wq = wqkv[hp % 2]
                for m, col0 in enumerate((256, 768, 1280)):
                    load("pool", wq.ap[:, m, :, :], w_in[li, :, col0 + hp * 128:col0 + (hp + 1) * 128].rearrange("(kc p) n -> p kc n", p=128), [wq.r(m)])

                def evac_q(t, t0, tw, b):
                    pg.add("act", ACT(qT.ap[:, t0:t0 + tw], ps[:, b, 0:tw], AF.Identity, scale=0.125), reads=[PB(b)], writes=[qT.r(t)])

                def evac_k(t, t0, tw, b):
                    pg.add("dve", lambda e, o=kTm[0].ap[0:64, t0:t0 + tw], i_=ps[0:64, b, 0:tw]: e.tensor_copy(o, i_), reads=[PB(b)], writes=[kTm[0].r(t)])
                    pg.add("act", lambda e, o=kTm[1].ap[64:128, t0:t0 + tw], i_=ps[64:128, b, 0:tw]: e.copy(o, i_), reads=[PB(b)], writes=[kTm[1].r(t)])
                inproj_T((lambda wq: lambda kc: wq.ap[:, 0, kc, :])(wq), wq.r(0), tiles_res, evac_q)
                inproj_T((lambda wq: lambda kc: wq.ap[:, 1, kc, :])(wq), wq.r(1), tiles_all, evac_k)
                for i4 in range(0, 18, 4):
                    b = nb()
                    n4 = min(4, 18 - i4)
                    fns = []
                    for ii in range(n4):
                        i = i4 + ii
                        for kc in range(8):
                            fns.append(MM(ps[:, b, ii * 128:(ii + 1) * 128], hT.ap[:, kc, i * 128:(i + 1) * 128], wq.ap[:, 2, kc, :],
                                          start=(kc == 0), stop=(kc == 7)))
                    pg.add("pe", seq(fns), reads=[wq.r(2)] + hT.rs([(kc, i4 // 4) for kc in range(8)]), writes=[PB(b)])
                    copy_op(Va.ap[:, i4:i4 + n4, :, 0:64], ps[:, b, 0:n4 * 128].rearrange("p (a h d) -> p a h d", a=n4, h=2),
                            [PB(b)], [Va.r(i4 // 4)])
                vregs_all = [Va.r("ones")] + Va.rs(range(5))
                for hh in range(2):
                    load("pool", biasb.ap[:, hh, :, :].rearrange("p a b -> p (a b)"), biasT_d[li, hp * 2 + hh], [biasb.r(hh)])
                for j in range(nqb):
                    isx = j < 16
                    kchunks = ([cs_of(j) + i for i in range(5)] + [16, 17]) if isx else [16, 17]
                    nk = len(kchunks)
                    ptr = []
                    for hh in range(2):
                        bp = npair()
                        fns = []
                        for ki, kc_ in enumerate(kchunks):
                            o_ = ps[:, bp + ki // 4, (ki % 4) * 128:(ki % 4 + 1) * 128]
                            if isx and ki < 5:
                                fns.append(MM(o_, kTm[hh].ap[:, kc_ * 128:(kc_ + 1) * 128], qT.ap[:, j * 128:(j + 1) * 128], start=True, stop=False))
                                fns.append(MM(o_, identb.ap, biasb.ap[:, hh, cls_of(j), ki * 128:(ki + 1) * 128], start=False, stop=True))
                            else:
                                fns.append(MM(o_, kTm[hh].ap[:, kc_ * 128:(kc_ + 1) * 128], qT.ap[:, j * 128:(j + 1) * 128]))
                        pg.add("pe", seq(fns), reads=[qT.r(j // 4), kTm[hh].r("z"), identb.r(), biasb.r(hh)] + kTm[hh].rs(sorted(set(k_ // 4 for k_ in kchunks))),
                               writes=[PB(bp), PB(bp + 1)])
                        pt = PT[hh]
                        sview = ps[:, bp:bp + 2, :].rearrange("p a b -> p (a b)")
                        if isx:
                            pg.add("act", ACT(pt.ap.rearrange("p a b -> p (a b)"), sview[:, 0:896], AF.Exp),
                                   reads=[PB(bp), PB(bp + 1)], writes=[pt.r("l"), pt.r("c")])
                            ptr.append([pt.r("l"), pt.r("c")])
                        else:
                            pg.add("act", ACT(pt.ap[:, 0:2, :].rearrange("p a b -> p (a b)"), sview[:, 0:256], AF.Exp),
                                   reads=[PB(bp), PB(bp + 1)], writes=[pt.r("l")])
                            ptr.append([pt.r("l")])
                    bo = nb()
                    for hh in range(2):
                        fns = [MM(ps[:, bo, hh * 65:(hh + 1) * 65], PT[hh].ap[:, ki, :], Va.ap[:, kc_, hh, :], start=(ki == 0), stop=(ki == nk - 1))
                               for ki, kc_ in enumerate(kchunks)]
                        pg.add("pe", seq(fns), reads=ptr[hh] + vregs_all, writes=[PB(bo)])
                    rc = rec[j % 2]
                    pv3 = ps[:, bo, 0:130].rearrange("p (h d) -> p h d", h=2)
                    pg.add("dve", RCP(rc.ap[:, 0:2].unsqueeze(2), pv3[:, :, 64:65]), reads=[PB(bo)], writes=[rc.r()])
                    pg.add("dve", TT(attn.ap[:, j, hp * 128:(hp + 1) * 128].rearrange("p (h d) -> p h d", h=2), pv3[:, :, 0:64],
                                     rc.ap[:, 0:2].unsqueeze(2).to_broadcast([128, 2, 64]), ALU.mult),
                           reads=[PB(bo), rc.r()], writes=[attn.r((j // 4, hp * 2)), attn.r((j // 4, hp * 2 + 1))])
            if li == 0:
                tap("attn", attn.ap.rearrange("p a b -> p (a b)"), [128, 18 * 512], BF16, attn.allregs())
            for (t, (t0, tw)) in tiles_res:
                nch = tw // 128
                for a in range(4):
                    b = nb()
                    pbv = ps[:, b, :].bitcast(BF16)
                    fns = [TR(pbv[:, ii * 128:(ii + 1) * 128], attn.ap[:, t * 4 + ii, a * 128:(a + 1) * 128], identb.ap) for ii in range(nch)]
                    pg.add("pe", seq(fns), reads=[identb.r()] + attn.rs([(t, 2 * a), (t, 2 * a + 1)]), writes=[PB(b)])
                    copy_op(attnT.ap[:, a, 0:tw], pbv[:, 0:tw], [PB(b)], [attnT.r(a)])
                outproj_partial(li, wo_a, 4, lambda k, t0, tw: attnT.ap[:, k, 0:tw], lambda t: attnT.rs(range(4)), [(t, (t0, tw))])
            if li == 0:
                tap("xM", xT.ap[:, 0, :], [128, NT], F32, xregs0())

        reset_scratch()
        n_ = nchunks_res
        NTL = NTILES[0] if not last else NTILES[1]
        lg = carve([128, 18, 36], F32)
        wr_sb = carve([128, 8, 36], F32)
        r_gmax = carve([128, 18], F32)
        r_goh = carve([128, 18, 4], F32)
        r_gex = carve([128, 18, 4], F32)
        r_gw = carve([128, 18], F32)
        r_em = carve([128, 18, 4, 8], F32)
        r_es = carve([128, 18, 8], F32)
        r_m1 = carve([128, 18], F32)
        r_o1 = carve([128, 18, 8], F32)
        r_e2 = carve([128, 18, 8], F32)
        r_m2 = carve([128, 18], F32)
        r_o2 = carve([128, 18, 8], F32)
        r_d = carve([128, 18], F32)
        r_w1 = carve([128, 18], F32)
        A1 = carve([128, 18, 4, 8], F32)
        A2 = carve([128, 18, 4, 8], F32)
        Mb = carve([128, 18, 32], BF16)
        rank = carve([128, 18, 32], F32)
        tmpk = carve([128, 18, 32], F32)
        cnt = carve([128, 32], F32)
        ntl = carve([128, 32], F32)
        pa = carve([128, 32], F32)
        pb_ = carve([128, 32], F32)
        segst = carve([128, 32], F32)
        posf = [carve([128, 18], F32) for _ in range(2)]
        cmpb = carve([128, 40, 32], F32)
        etl = carve([128, 40], F32)
        htm = [carve([128, D], BF16) for _ in range(2)]

        carve_norm()
        load("sp", wr_sb.ap, wr_d[li].rearrange("(kc p) n -> p kc n", p=128), [wr_sb.r()])
        norm_phase(li, 1, tiles_res, router=(wr_sb, lg))

        def V(b_):
            return b_.ap[:, 0:n_]
        G = lg.ap[:, 0:n_, 0:4]
        E4 = lg.ap[:, 0:n_, 4:36].rearrange("p n (g e) -> p n g e", g=4)

        def dv(fn, reads, writes):
            pg.add("dve", fn, reads=[x_.r() for x_ in reads], writes=[x_.r() for x_ in writes])

        def bc3(b_, k):
            return V(b_).unsqueeze(2).to_broadcast([128, n_, k])

        pw = [pw1[li], pw2[li]]
        dv(RED(V(r_gmax), G, ALU.max), [lg], [r_gmax])
        dv(TT(V(r_goh), G, bc3(r_gmax, 4), ALU.is_equal), [lg, r_gmax], [r_goh])
        dv(TT(V(r_gex), G, bc3(r_gmax, 4), ALU.subtract), [lg, r_gmax], [r_gex])
        pg.add("act", ACT(V(r_gex), V(r_gex), AF.Exp), reads=[r_gex.r()], writes=[r_gex.r()])
        dv(RED(V(r_gw), V(r_gex), ALU.add), [r_gex], [r_gw])
        dv(RCP(V(r_gw), V(r_gw)), [r_gw], [r_gw])
        dv(TT(V(r_em), E4, V(r_goh).unsqueeze(3).to_broadcast([128, n_, 4, 8]), ALU.mult), [lg, r_goh], [r_em])
        dv(RED(V(r_es), V(r_em).rearrange("p n g e -> p n e g"), ALU.add), [r_em], [r_es])
        dv(RED(V(r_m1), V(r_es), ALU.max), [r_es], [r_m1])
        dv(TT(V(r_o1), V(r_es), bc3(r_m1, 8), ALU.is_equal), [r_es, r_m1], [r_o1])
        dv(STT(V(r_e2), V(r_o1), -1e30, V(r_es), ALU.mult, ALU.add), [r_o1, r_es], [r_e2])
        dv(RED(V(r_m2), V(r_e2), ALU.max), [r_e2], [r_m2])
        dv(TT(V(r_o2), V(r_e2), bc3(r_m2, 8), ALU.is_equal), [r_e2, r_m2], [r_o2])
        dv(TT(V(r_d), V(r_m2), V(r_m1), ALU.subtract), [r_m1, r_m2], [r_d])
        pg.add("act", ACT(V(r_d), V(r_d), AF.Exp), reads=[r_d.r()], writes=[r_d.r()])
        dv(TS(V(r_w1), V(r_d), 1.0, ALU.add), [r_d], [r_w1])
        dv(RCP(V(r_w1), V(r_w1)), [r_w1], [r_w1])
        dv(TT(V(pw[1]), V(r_d), V(r_w1), ALU.mult), [r_d, r_w1], [pw[1]])
        dv(TT(V(pw[0]), V(r_w1), V(r_gw), ALU.mult), [r_w1, r_gw], [pw[0]])
        dv(TT(V(pw[1]), V(pw[1]), V(r_gw), ALU.mult), [pw[1], r_gw], [pw[1]])
        gohb = V(r_goh).unsqueeze(3).to_broadcast([128, n_, 4, 8])
        dv(TT(V(A1), gohb, V(r_o1).unsqueeze(2).to_broadcast([128, n_, 4, 8]), ALU.mult), [r_goh, r_o1], [A1])
        dv(TT(V(A2), gohb, V(r_o2).unsqueeze(2).to_broadcast([128, n_, 4, 8]), ALU.mult), [r_goh, r_o2], [A2])
        A1f = V(A1).rearrange("p n g e -> p n (g e)")
        A2f = V(A2).rearrange("p n g e -> p n (g e)")
        dv(TT(V(Mb), A1f, A2f, ALU.add), [A1, A2], [Mb])
        if li == 0:
            tap("lg", lg.ap.rearrange("p a b -> p (a b)"), [128, 18 * 36], F32, [lg.r()])
        brk = [nb(), nb()]
        for bi in range(2):
            lo, hi = bi * 16, min(n_, bi * 16 + 16)
            if lo >= hi:
                continue
            fns = []
            for i in range(lo, hi):
                col = (i - lo) * 32
                for i2 in range(i):
                    fns.append(MM(ps[:, brk[bi], col:col + 32], onesb.ap, Mb.ap[:, i2, :], start=(i2 == 0), stop=False))
                fns.append(MM(ps[:, brk[bi], col:col + 32], Ub.ap, Mb.ap[:, i, :], start=(i == 0), stop=True))
            pg.add("pe", seq(fns), reads=[onesb.r(), Ub.r(), Mb.r()], writes=[PB(brk[bi])])
            copy_op(rank.ap[:, lo:hi, :], ps[:, brk[bi], 0:(hi - lo) * 32].rearrange("p (a b) -> p a b", b=32), [PB(brk[bi])], [rank.r(bi)])
        bcn = nb()
        pg.add("pe", seq([MM(ps[:, bcn, 0:32], onesb.ap, Mb.ap[:, i, :], start=(i == 0), stop=(i == n_ - 1)) for i in range(n_)]),
               reads=[onesb.r(), Mb.r()], writes=[PB(bcn)])
        pg.add("dve", lambda e, o=cnt.ap, i_=ps[:, bcn, 0:32]: e.tensor_copy(o, i_), reads=[PB(bcn)], writes=[cnt.r()])
        dv(TS(ntl.ap, cnt.ap, 0.0, ALU.is_gt), [cnt], [ntl])
        for m in range(1, 5):
            dv(STT(ntl.ap, cnt.ap, 512.0 * m, ntl.ap, ALU.is_gt, ALU.add), [cnt, ntl], [ntl])
        src, dst = ntl, pa
        for sh in (1, 2, 4, 8, 16):
            dv(lambda e, o=dst.ap[:, 0:sh], i_=src.ap[:, 0:sh]: e.tensor_copy(o, i_), [src], [dst])
            dv(TT(dst.ap[:, sh:32], src.ap[:, sh:32], src.ap[:, 0:32 - sh], ALU.add), [src, dst], [dst])
            src, dst = dst, (pb_ if dst is pa else pa)
        incl = src
        dv(TT(segst.ap, incl.ap, ntl.ap, ALU.subtract), [incl, ntl], [segst])
        dv(TS(segst.ap, segst.ap, 512.0, ALU.mult), [segst], [segst])
        pg.add("dve", TT(V(rank), V(rank), segst.ap.unsqueeze(1).to_broadcast([128, n_, 32]), ALU.add),
               reads=[rank.r(0), rank.r(1), segst.r()], writes=[rank.r(0), rank.r(1)])
        for k, Af in ((0, A1f), (1, A2f)):
            pg.add("dve", TT(V(tmpk), V(rank), Af, ALU.mult), reads=rank.allregs() + [A1.r(), A2.r()], writes=[tmpk.r()])
            dv(RED(V(posf[k]), V(tmpk), ALU.add), [tmpk], [posf[k]])
            dv(lambda e, o=V(posi[li][k]), i_=V(posf[k]): e.tensor_copy(o, i_), [posf[k]], [posi[li][k]])
        dv(TT(cmpb.ap[:, 0:NTL, :], incl.ap.unsqueeze(1).to_broadcast([128, NTL, 32]), tau.ap[:, 0:NTL].unsqueeze(2).to_broadcast([128, NTL, 32]), ALU.is_le),
           [incl, tau], [cmpb])
        dv(RED(etl.ap[:, 0:NTL], cmpb.ap[:, 0:NTL, :], ALU.add), [cmpb], [etl])
        dv(TS(etl.ap[:, 0:NTL], etl.ap[:, 0:NTL], 31.0, ALU.min), [etl], [etl])
        dv(lambda e, o=etl.ap[:, 0:NTL], c_=pconst.ap[:, li:li + 1]: e.tensor_scalar(out=o, in0=o, scalar1=128.0, scalar2=c_, op0=ALU.mult, op1=ALU.add),
           [etl, pconst], [etl])
        dv(lambda e, o=widx[li].ap[:, 0:NTL], i_=etl.ap[:, 0:NTL]: e.tensor_copy(o, i_), [etl], [widx[li]])
        if li == 0:
            tap("pos1", posf[0].ap, [128, 18], F32, [posf[0].r()])
            tap("pos2", posf[1].ap, [128, 18], F32, [posf[1].r()])
            tap("etl", etl.ap, [128, 40], F32, [etl.r()])
        HsL = Buf(Hs_ap, parents=[HsPrev[0]])
        for i in range(n_):
            b = nb()
            pbv = ps[:, b, :].bitcast(BF16)
            fns = [TR(pbv[:, kc * 128:(kc + 1) * 128], hT.ap[:, kc, i * 128:(i + 1) * 128], identb.ap) for kc in range(8)]
            pg.add("pe", seq(fns), reads=[identb.r()] + hT.rs([(kc, i // 4) for kc in range(8)]), writes=[PB(b)])
            hb_ = htm[i % 2]
            copy_op(hb_.ap, pbv, [PB(b)], [hb_.r()])
            for k in range(2):
                pg.add("pool", lambda e, i_=hb_.ap, off=posi[li][k].ap[:, i:i + 1]: e.indirect_dma_start(
                    out=Hs_ap, out_offset=bass.IndirectOffsetOnAxis(ap=off, axis=0), in_=i_, in_offset=None),
                    reads=[hb_.r(), posi[li][k].r()], writes=[HsL.r((i, k))], dma=True)
        HsPrev[0] = HsL
        hs_regs = HsL.allregs()

        reset_scratch()
        wgu = [carve([128, 8192], BF16) for _ in range(2)]
        wdn = [carve([128, 4096], BF16) for _ in range(2)]
        hid = [carve([128, 4, 512], BF16) for _ in range(2)]
        sg = [carve([128, 512], BF16) for _ in range(3)]
        rst = [carve([128, D], F32) for _ in range(2)]
        hg = [Buf(hT_t[:, q * 4096:(q + 1) * 4096].rearrange("p (a b) -> p a b", a=8), parents=[hT]) for q in range(2)]
        hst = [Buf(hT_t[:, 8192 + q * 4096:8192 + (q + 1) * 4096].rearrange("p (a b) -> p a b", a=4), parents=[hT]) for q in range(2)]
        RL = Buf(R_ap, parents=[RPrev[0]])

        def load_dn(tt):
            ws = wdn[tt % 2]
            pg.add("pool", lambda e, o=ws.ap, off=widx[li].ap[:, tt:tt + 1]: e.indirect_dma_start(
                out=o, out_offset=None, in_=W_dn, in_offset=bass.IndirectOffsetOnAxis(ap=off, axis=0)),
                reads=[widx[li].r()], writes=[ws.r()], dma=True)

        def tile_loads(tt):
            ws = wgu[tt % 2]
            pg.add("pool", lambda e, o=ws.ap, off=widx[li].ap[:, tt:tt + 1]: e.indirect_dma_start(
                out=o, out_offset=None, in_=W_gu, in_offset=bass.IndirectOffsetOnAxis(ap=off, axis=0)),
                reads=[widx[li].r()], writes=[ws.r()], dma=True)
            hs_ = hst[tt % 2]
            pg.add("sp", lambda e, o=hs_.ap, i_=Hs_ap[tt * 512:(tt + 1) * 512, :].rearrange("(c p) n -> p c n", p=128): e.dma_start(out=o, in_=i_),
                   reads=hs_regs, writes=[hs_.r()], dma=True)

        def tile_front(tt):
            ws = wgu[tt % 2]
            hs_ = hst[tt % 2]
            hg_ = hg[tt % 2]
            hb = hid[tt % 2]
            for kc in range(8):
                b = nb()
                pbv = ps[:, b, :].bitcast(BF16)
                fns = [TR(pbv[:, c * 128:(c + 1) * 128], hs_.ap[:, c, kc * 128:(kc + 1) * 128], identb.ap) for c in range(4)]
                pg.add("pe", seq(fns), reads=[identb.r(), hs_.r()], writes=[PB(b)])
                copy_op(hg_.ap[:, kc, :], pbv[:, 0:512], [PB(b)], [hg_.r(kc)])
            hregs = hg_.rs(range(8))
            wg_ = ws.ap[:, 0:4096].rearrange("p (a b) -> p a b", a=8)
            wu_ = ws.ap[:, 4096:8192].rearrange("p (a b) -> p a b", a=8)
            for dc in range(4):
                bg = nb()
                bu = nb()
                fg = [MM(ps[:, bg, :], wg_[:, kc, dc * 128:(dc + 1) * 128], hg_.ap[:, kc, :], start=(kc == 0), stop=(kc == 7)) for kc in range(8)]
                fu = [MM(ps[:, bu, :], wu_[:, kc, dc * 128:(dc + 1) * 128], hg_.ap[:, kc, :], start=(kc == 0), stop=(kc == 7)) for kc in range(8)]
                pg.add("pe", seq(fg), reads=[ws.r()] + hregs, writes=[PB(bg)])
                pg.add("pe", seq(fu), reads=[ws.r()] + hregs, writes=[PB(bu)])
                sgb = sg[dc % 3]
                pg.add("act", ACT(sgb.ap, ps[:, bg, :], AF.Silu), reads=[PB(bg)], writes=[sgb.r()])
                pg.add("dve", TT(hb.ap[:, dc, :], sgb.ap, ps[:, bu, :], ALU.mult), reads=[sgb.r(), PB(bu)], writes=[hb.r(dc)])

        def tile_back(tt):
            ws = wdn[tt % 2]
            hb = hid[tt % 2]
            wd_ = ws.ap.rearrange("p (a b) -> p a b", a=4)
            for sc_ in range(4):
                ro = rst[sc_ % 2]
                for half in range(2):
                    b = nb()
                    fns = [MM(ps[:, b, :], hb.ap[:, dc, sc_ * 128:(sc_ + 1) * 128], wd_[:, dc, half * 512:(half + 1) * 512], start=(dc == 0), stop=(dc == 3))
                           for dc in range(4)]
                    pg.add("pe", seq(fns), reads=[ws.r()] + hb.rs(range(4)), writes=[PB(b)])
                    copy_op(ro.ap[:, half * 512:(half + 1) * 512], ps[:, b, :], [PB(b)], [ro.r(half)])
                r0 = tt * 512 + sc_ * 128
                pg.add("sp", lambda e, o=R_ap[r0:r0 + 128, :], i_=ro.ap: e.dma_start(out=o, in_=i_), reads=[ro.r(0), ro.r(1)], writes=[RL.r((tt, sc_))], dma=True)

        tile_loads(0)
        load_dn(0)
        for tt in range(NTL):
            if tt + 1 < NTL:
                tile_loads(tt + 1)
            tile_front(tt)
            if tt > 0:
                tile_back(tt - 1)
            if tt + 1 < NTL:
                load_dn(tt + 1)
        tile_back(NTL - 1)
        RPrev[0] = RL
        r_regs = RL.allregs()

        reset_scratch()
        Gb = [[carve([128, D], F32) for _ in range(2)] for _ in range(2)]
        yb = [carve([128, D], F32) for _ in range(2)]
        for i in range(n_):
            t = i // 4
            s = 0 if t < 4 else 1
            for k in range(2):
                g_ = Gb[k][i % 2]
                pg.add("pool", lambda e, o=g_.ap, off=posi[li][k].ap[:, i:i + 1]: e.indirect_dma_start(
                    out=o, out_offset=None, in_=R_ap, in_offset=bass.IndirectOffsetOnAxis(ap=off, axis=0)),
                    reads=r_regs + [posi[li][k].r()], writes=[g_.r()], dma=True)
            y_ = yb[i % 2]
            pg.add("dve", TS(y_.ap, Gb[0][i % 2].ap, pw[0].ap[:, i:i + 1], ALU.mult), reads=[Gb[0][i % 2].r(), pw[0].r()], writes=[y_.r()])
            pg.add("dve", STT(y_.ap, Gb[1][i % 2].ap, pw[1].ap[:, i:i + 1], y_.ap, ALU.mult, ALU.add), reads=[Gb[1][i % 2].r(), pw[1].r(), y_.r()], writes=[y_.r()])
            for half in range(2):
                b = nb()
                fns = [TR(ps[:, b, c4 * 128:(c4 + 1) * 128], y_.ap[:, (half * 4 + c4) * 128:(half * 4 + c4 + 1) * 128], ident.ap) for c4 in range(4)]
                pg.add("pe", seq(fns), reads=[ident.r(), y_.r()], writes=[PB(b)])
                for c4 in range(4):
                    j = half * 4 + c4
                    pg.add("dve", STT(xT.ap[:, j, i * 128:(i + 1) * 128], ps[:, b, c4 * 128:(c4 + 1) * 128], mod.ap[:, li, MG2 + j, s:s + 1],
                                      xT.ap[:, j, i * 128:(i + 1) * 128], ALU.mult, ALU.add),
                           reads=[PB(b), mod.r(li), xT.r((j, t))], writes=[xT.r((j, t))])
        for g in hT.allregs():
            _inherit(g, [r_ for b_ in hg + hst for r_ in b_.allregs()])
        if li == 0:
            tap("x1", xT.ap[:, 0, :], [128, NT], F32, xregs0())

    reset_scratch()
    yT = carve([128, 8, 512], F32)
    ob = [carve([128, D], F32) for _ in range(2)]
    carve_norm()
    nfb = 32
    for (t, (t0, tw)) in list(enumerate(TILES))[:4]:
        rs_ = rms_rstd(t, t0, tw)
        for c in range(8):
            pg.add("dve", STT(yT.ap[:, c, 0:tw], xT.ap[:, c, t0:t0 + tw], nrm.ap[:, nfb + c:nfb + c + 1], rs_.ap[:, 0:tw], ALU.mult, ALU.mult),
                   reads=[xT.r((c, t)), nrm.r(), rs_.r()], writes=[yT.r(c)])
        for ii in range(4):
            i = t * 4 + ii
            o_ = ob[i % 2]
            for half in range(2):
                b = nb()
                fns = [TR(ps[:, b, c4 * 128:(c4 + 1) * 128], yT.ap[:, half * 4 + c4, ii * 128:(ii + 1) * 128], ident.ap) for c4 in range(4)]
                pg.add("pe", seq(fns), reads=[ident.r()] + yT.rs(range(half * 4, half * 4 + 4)), writes=[PB(b)])
                copy_op(o_.ap[:, half * 512:(half + 1) * 512], ps[:, b, :], [PB(b)], [o_.r(half)])
            op = pg.add("sp", lambda e, o=out_d[i * 128:(i + 1) * 128, :], i_=o_.ap: e.dma_start(out=o, in_=i_), reads=[o_.r(0), o_.r(1)], dma=True)
            pg.final_dmas.append(op)

    pg.emit(st)
    st.close()
    return nc, tap_out


_CONST = {}


def _constants():
    if _CONST:
        return _CONST
    bf = ml_dtypes.bfloat16
    n = np.arange(NX)
    ang = 2.0 * np.pi * ((n[:, None] * n[None, :]) % NX).astype(np.float64) / NX
    _CONST["CN"] = (np.cos(ang) / np.sqrt(NX)).astype(np.float32).astype(bf)
    _CONST["SN"] = (-np.sin(ang) / np.sqrt(NX)).astype(np.float32).astype(bf)
    m = np.arange(256)
    ang2 = 2.0 * np.pi * ((m[:, None] * m[None, :]) % 256).astype(np.float64) / 256
    _CONST["C2"] = (np.cos(ang2) / 16.0).astype(np.float32).astype(bf)
    _CONST["S2"] = (-np.sin(ang2) / 16.0).astype(np.float32).astype(bf)
    c = np.arange(64)
    angc = 2.0 * np.pi * ((c[:, None] * c[None, :]) % 64).astype(np.float64) / 64
    bc = np.zeros((256, 256), np.float32)
    bs = np.zeros((256, 256), np.float32)
    for g in range(4):
        bc[g * 64:(g + 1) * 64, g * 64:(g + 1) * 64] = np.cos(angc) / 8.0
        bs[g * 64:(g + 1) * 64, g * 64:(g + 1) * 64] = np.sin(angc) / 8.0
    _CONST["BCS"] = np.concatenate([bc, bs], axis=1)
    _CONST["ident"] = np.eye(128, dtype=np.float32)
    _CONST["Uc"] = np.triu(np.ones((128, 128), np.float32), k=1)
    _CONST["tau"] = np.ascontiguousarray(np.broadcast_to(np.arange(40, dtype=np.float32)[None, :], (128, 40)))
    _CONST["pconst"] = np.stack([np.arange(128, dtype=np.float32), np.arange(128, dtype=np.float32) + 4096.0], axis=1)
    ro = np.zeros((5, 128, 5, 128), np.int64)
    co = np.zeros((5, 128, 5, 128), np.int64)
    va = np.zeros((5, 128, 5, 128), bool)
    kk = np.arange(128)
    kr, kcol = kk // 64, kk % 64
    qr, qc = kk // 64, kk % 64
    for cl, j in enumerate((0, 1, 2, 14, 15)):
        r = 2 * j + qr
        rstart = np.clip(r - 4, 0, 24)
        cstart = np.clip(qc - 8, 0, 48)
        for i in range(5):
            krow = 2 * (cs_of(j) + i) + kr
            okr = (krow[:, None] >= rstart[None, :]) & (krow[:, None] < rstart[None, :] + 8)
            okc = (kcol[:, None] >= cstart[None, :]) & (kcol[:, None] < cstart[None, :] + 16)
            va[cl, :, i, :] = okr & okc
            ro[cl, :, i, :] = np.clip(krow[:, None] - r[None, :] + 7, 0, 14)
            co[cl, :, i, :] = np.clip(kcol[:, None] - qc[None, :] + 15, 0, 30)
    _CONST["ro"], _CONST["co"], _CONST["va"] = ro, co, va
    return _CONST


def _fm(v):
    v = np.asarray(v, np.float32)
    return np.ascontiguousarray(v.reshape(-1, 128).T)


def prepare_inputs(x, c, ctx, c_ctx, w_ada, b_ada, norm1, norm2, w_in, w_fourier, w_conv, rpb, w_out,
                   w_rg, b_rg, w_re, b_re, w_gate, w_up, w_down, norm_final):
    K = _constants()
    f32 = lambda a: np.ascontiguousarray(np.asarray(a, np.float32))
    shared = {
        "w_ada": f32(w_ada), "w_in": f32(w_in), "w_out": f32(w_out), "wf": f32(w_fourier),
        "CN": K["CN"], "SN": K["SN"], "C2": K["C2"], "S2": K["S2"], "BCS": K["BCS"], "ident": K["ident"],
    }
    shared["bada"] = np.concatenate([_fm(b_ada[0]), _fm(b_ada[1])], axis=1)
    shared["nrm"] = np.concatenate([_fm(norm1[0]), _fm(norm2[0]), _fm(norm1[1]), _fm(norm2[1]), _fm(norm_final)], axis=1)
    wc = np.asarray(w_conv, np.float32)
    shared["wconv"] = np.ascontiguousarray(wc.reshape(2, 3, 2, 128).transpose(3, 0, 2, 1).reshape(128, 12))
    shared["wr"] = np.ascontiguousarray(np.concatenate([np.asarray(w_rg, np.float32), np.asarray(w_re, np.float32)], axis=2))
    brv = np.concatenate([np.asarray(b_rg, np.float32), np.asarray(b_re, np.float32)], axis=1).reshape(1, 72)
    shared["br"] = np.ascontiguousarray(np.broadcast_to(brv, (128, 72)))
    rp = np.asarray(rpb, np.float32)
    g = rp[:, :, K["ro"], K["co"]]
    g = np.where(K["va"][None, None], g, np.float32(-30000.0))
    shared["biasT"] = np.ascontiguousarray(g.transpose(0, 1, 3, 2, 4, 5).reshape(2, 8, 128, 3200).astype(np.float32))
    wgu = np.empty((2, NE, 128, 8192), np.float32)
    wgu[..., 0:4096] = np.asarray(w_gate, np.float32).reshape(2, NE, 8, 128, 512).transpose(0, 1, 3, 2, 4).reshape(2, NE, 128, 4096)
    wgu[..., 4096:8192] = np.asarray(w_up, np.float32).reshape(2, NE, 8, 128, 512).transpose(0, 1, 3, 2, 4).reshape(2, NE, 128, 4096)
    shared["W_gu"] = wgu.reshape(2 * NE * 128, 8192)
    shared["W_dn"] = np.ascontiguousarray(
        np.asarray(w_down, np.float32).reshape(2, NE, 4, 128, 1024).transpose(0, 1, 3, 2, 4).reshape(2 * NE * 128, 4096))
    shared["Uc"] = K["Uc"]
    shared["tau"] = K["tau"]
    shared["pconst"] = K["pconst"]
    in_maps = []
    for b in range(8):
        m = dict(shared)
        m["xin"] = np.ascontiguousarray(np.concatenate([np.asarray(x[b], np.float32), np.asarray(ctx[b], np.float32)], axis=0))
        cvm = np.stack([_fm(c[b]), _fm(c_ctx)], axis=2).reshape(128, 16)
        m["cvec"] = np.ascontiguousarray(cvm)
        in_maps.append(m)
    return in_maps


_PROG = {}


def kernel(**inputs):
    if "nc" not in _PROG:
        _PROG["nc"] = build_program()[0]
    in_maps = prepare_inputs(**inputs)
    res = run_bass_kernel_spmd(_PROG["nc"], in_maps, core_ids=list(range(8)))
    return np.stack([np.asarray(r["out"], np.float32) for r in res.results], axis=0)
```

```python
import numpy as np
import ml_dtypes
from contextlib import ExitStack
import concourse.bass as bass
import concourse.mybir as mybir
from concourse.bass_utils import run_bass_kernel_spmd

F32 = mybir.dt.float32
BF16 = mybir.dt.bfloat16
U8 = mybir.dt.uint8
I32 = mybir.dt.int32
AF = mybir.ActivationFunctionType
ALU = mybir.AluOpType
AX = mybir.AxisListType

COMPUTE = ("pe", "act", "dve", "pool")
import os
SAME_ENGINE_SYNC = os.environ.get('SES', '1') == '1'
ATT_SKEW = os.environ.get('ATT_SKEW', '0') == '1'


class Reg:
    __slots__ = ("writer", "rd", "rdma")

    def __init__(self):
        self.writer = None
        self.rd = {}
        self.rdma = []


def _inherit(g, parents):
    for p in parents:
        cands = list(p.rd.values())
        if p.writer is not None:
            cands.append(p.writer)
        for op in cands:
            if op.dma:
                g.rdma.append(op)
            else:
                o = g.rd.get(op.eng)
                if o is None or o.pos < op.pos:
                    g.rd[op.eng] = op
        g.rdma.extend(p.rdma)


class Buf:
    def __init__(self, ap, parents=()):
        self.ap = ap
        self.parents = list(parents)
        self.regs = {}

    def r(self, key=0):
        g = self.regs.get(key)
        if g is None:
            g = Reg()
            for pb in self.parents:
                _inherit(g, pb.allregs())
            self.regs[key] = g
        return g

    def rs(self, keys):
        return [self.r(k) for k in keys]

    def allregs(self):
        return list(self.regs.values())


class Op:
    __slots__ = ("eng", "fn", "deps", "dma", "pos", "waits", "signal", "dsem", "dval", "dprev")


class Prog:
    def __init__(self, nc, n_dma_sems=20):
        self.nc = nc
        self.ops = []
        self.streams = {e: [] for e in ("pe", "act", "dve", "pool", "sp")}
        self.n_dma_sems = n_dma_sems
        self.dma_rr = {"sp": 0, "pool": 0, "act": 0}
        self.dma_tot = {}
        self.final_dmas = []

    def add(self, eng, fn, reads=(), writes=(), dma=False):
        op = Op()
        op.eng = eng
        op.fn = fn
        op.dma = dma
        op.waits = []
        op.signal = False
        deps = []
        for r in reads:
            if r.writer is not None:
                deps.append(r.writer)
        for w in writes:
            if w.writer is not None:
                deps.append(w.writer)
            deps.extend(w.rd.values())
            deps.extend(w.rdma)
        for r in reads:
            if dma:
                r.rdma.append(op)
            else:
                r.rd[eng] = op
        for w in writes:
            w.writer = op
            w.rd = {}
            w.rdma = []
        op.deps = deps
        op.pos = len(self.streams[eng])
        self.streams[eng].append(op)
        self.ops.append(op)
        if dma:
            k = self.dma_rr[eng]
            self.dma_rr[eng] = (k + 1) % self.n_dma_sems
            key = (eng, k)
            prev = self.dma_tot.get(key, 0)
            op.dsem = key
            op.dprev = prev
            op.dval = prev + 16
            self.dma_tot[key] = prev + 16
        return op

    def emit(self, stack):
        nc = self.nc
        sems = {e: stack.enter_context(nc.semaphore("s_" + e)) for e in COMPUTE}
        dsems = {}
        for key in self.dma_tot:
            dsems[key] = stack.enter_context(nc.semaphore("d_%s%d" % key))
        waited = {c: {} for c in self.streams}
        for op in self.ops:
            c = op.eng
            w = waited[c]
            best = {}
            for d in op.deps:
                if d is op:
                    continue
                if d.dma:
                    if w.get(d.dsem, 0) < d.dval:
                        w[d.dsem] = d.dval
                        op.waits.append(("d", d.dsem, d.dval))
                else:
                    if d.eng == c and (c == "pe" or not SAME_ENGINE_SYNC):
                        continue
                    if d.pos > best.get(d.eng, -1):
                        best[d.eng] = d.pos
            for e, p in best.items():
                if w.get(e, -1) < p:
                    w[e] = p
                    prod = self.streams[e][p]
                    prod.signal = True
                    op.waits.append(("c", e, prod))
            if op.dma and op.dprev > 0:
                if w.get(op.dsem, 0) < op.dprev:
                    w[op.dsem] = op.dprev
                    op.waits.append(("d", op.dsem, op.dprev))
            op.deps = None
        for e in COMPUTE:
            cnt = 0
            for op in self.streams[e]:
                if op.signal and not op.dma:
                    cnt += 1
                    op.dval = cnt
        final = list(self.final_dmas)

        def run_stream(engname, eng):
            for op in self.streams[engname]:
                for kind, key, v in op.waits:
                    if kind == "d":
                        eng.wait_ge(dsems[key], v)
                    else:
                        eng.wait_ge(sems[key], v.dval)
                ins = op.fn(eng)
                if op.dma:
                    ins.then_inc(dsems[op.dsem], 16)
                elif op.signal:
                    ins.then_inc(sems[engname], 1)
            if engname == "sp":
                for op in final:
                    eng.wait_ge(dsems[op.dsem], op.dval)

        with nc.Block() as block:
            @block.sync
            def _(e):
                run_stream("sp", e)

            @block.tensor
            def _(e):
                run_stream("pe", e)

            @block.scalar
            def _(e):
                run_stream("act", e)

            @block.vector
            def _(e):
                run_stream("dve", e)

            @block.gpsimd
            def _(e):
                run_stream("pool", e)


def seq(fns):
    def f(e):
        ins = None
        for g in fns:
            ins = g(e)
        return ins
    return f


D = 1024
NX = 2048
NC_ = 256
NT = NX + NC_
NE = 32
EPS = 1e-6
TILES = [(0, 512), (512, 512), (1024, 512), (1536, 512), (2048, 256)]
SCR_BYTES = 97400
TW = int(os.environ.get("TW", "384"))
NSC = TW // 128
NTILES = ((4608 + 32 * (TW - 1)) // TW + int(os.environ.get("XT", "0")), (4096 + 32 * (TW - 1)) // TW + int(os.environ.get("XT", "0")))
NTMAX = NTILES[0]
NSLOT = NTMAX * TW


def cs_of(j):
    return min(max(j - 2, 0), 11)


def cls_of(j):
    return {0: 0, 1: 1, 14: 3, 15: 4}.get(j, 2)


def MM(out, lhsT, rhs, start=True, stop=True):
    return lambda e: e.matmul(out, lhsT, rhs, start=start, stop=stop)


def TR(out, in_, idn):
    return lambda e: e.transpose(out, in_, idn)


def ACT(out, in_, func, **kw):
    return lambda e: e.activation(out=out, in_=in_, func=func, **kw)


def TT(out, in0, in1, op):
    return lambda e: e.tensor_tensor(out=out, in0=in0, in1=in1, op=op)


def STT(out, in0, scalar, in1, op0, op1):
    return lambda e: e.scalar_tensor_tensor(out=out, in0=in0, scalar=scalar, in1=in1, op0=op0, op1=op1)


def TS(out, in0, s1, op0):
    return lambda e: e.tensor_scalar(out=out, in0=in0, scalar1=s1, scalar2=None, op0=op0)


def RCP(out, in_):
    return lambda e: e.reciprocal(out=out, in_=in_)


def RED(out, in_, op):
    return lambda e: e.tensor_reduce(out=out, in_=in_, axis=AX.X, op=op)


def MEMSET(ap, v):
    return lambda e: e.memset(ap, v)


def build_program(n_layers=2, taps=(), moe_experts=NE, do_mixer=True):
    nc = bass.Bass("TRN2", target_bir_lowering=False)
    st = ExitStack()
    pg = Prog(nc)
    tapset = set(taps)
    tap_out = {}

    def dram(name, shape, dt=F32, out=False):
        return nc.dram_tensor(name, list(shape), dt, kind="ExternalOutput" if out else "ExternalInput").ap()

    xin = dram("xin", [NT, D])
    cvec_d = dram("cvec", [128, 16])
    w_ada = dram("w_ada", [2, D, 6 * D])
    bada_d = dram("bada", [128, 96])
    nrm_d = dram("nrm", [128, 40])
    w_in = dram("w_in", [2, D, 2560])
    w_out = dram("w_out", [2, D, D])
    wf_d = dram("wf", [2, 256, 256])
    wconv_d = dram("wconv", [128, 12])
    wr_d = dram("wr", [2, D, 36])
    br_d = dram("br", [128, 72])
    W_gu = dram("W_gu", [2 * NE * 128, 8192])
    W_dn = dram("W_dn", [2 * NE * 128, 4096])
    U_d = dram("Uc", [128, 128])
    tau_d = dram("tau", [128, NTMAX])
    pconst_d = dram("pconst", [128, 2])
    Hs_ap = nc.dram_tensor("Hs", [NSLOT, D], BF16, kind="Internal").ap()
    R_ap = nc.dram_tensor("Rr", [NSLOT, D], F32, kind="Internal").ap()
    biasT_d = dram("biasT", [2, 8, 128, 3200])
    CN_d = dram("CN", [NX, NX], BF16)
    SN_d = dram("SN", [NX, NX], BF16)
    C2_d = dram("C2", [256, 256], BF16)
    S2_d = dram("S2", [256, 256], BF16)
    BCS_d = dram("BCS", [256, 512])
    ident_d = dram("ident", [128, 128])
    out_d = dram("out", [NX, D], out=True)

    def sbt(name, shape, dt=F32):
        return st.enter_context(nc.sbuf_tensor(name, list(shape), dt))

    xT = Buf(sbt("xT", [128, 8, NT])[:])
    hT_t = sbt("hT", [128, 8 * NT], BF16)
    hT = Buf(hT_t[:].rearrange("p (c t) -> p c t", c=8))
    ident = Buf(sbt("ident_sb", [128, 128])[:])
    identb = Buf(sbt("identb", [128, 128], BF16)[:])
    onesb = Buf(sbt("onesb", [128, 128], BF16)[:])
    cv = Buf(sbt("cv", [128, 16])[:])
    scv = Buf(sbt("scv", [128, 16])[:])
    bada = Buf(sbt("bada_sb", [128, 96])[:])
    nrm = Buf(sbt("nrm_sb", [128, 40])[:])
    wconv = Buf(sbt("wconv_sb", [128, 12])[:])
    brs = Buf(sbt("br_sb", [128, 72])[:])
    mod = Buf(sbt("mod", [128, 2, 48, 2])[:])
    gm = Buf(sbt("gm", [128, 2, 2, 8, 2])[:])
    Ub = Buf(sbt("Ub", [128, 128], BF16)[:])
    tau = Buf(sbt("tau_sb", [128, NTMAX])[:])
    pconst = Buf(sbt("pconst_sb", [128, 2])[:])
    pw1 = [Buf(sbt("pw1_%d" % l_, [128, 18])[:]) for l_ in range(2)]
    pw2 = [Buf(sbt("pw2_%d" % l_, [128, 18])[:]) for l_ in range(2)]
    posi = [[Buf(sbt("posi%d_%d" % (l_, k_), [128, 18], I32)[:]) for k_ in range(2)] for l_ in range(2)]
    widx = [Buf(sbt("widx%d" % l_, [128, NTMAX], I32)[:]) for l_ in range(2)]
    scr_t = sbt("scr", [128, SCR_BYTES], U8)
    ps_t = st.enter_context(nc.psum_tensor("ps", [128, 8, 512], F32))
    PS = Buf(ps_t[:])
    ps = PS.ap

    allocs = []
    cur = [0]

    def reset_scratch():
        cur[0] = 0

    def carve(shape, dt, at=None):
        esz = 4 if dt == F32 else 2
        n = 1
        for s_ in shape[1:]:
            n *= s_
        nbytes = (n * esz + 31) // 32 * 32
        off = cur[0] if at is None else at
        if at is None:
            cur[0] = off + nbytes
        assert off + nbytes <= SCR_BYTES, ("scratch overflow", off + nbytes)
        ap = scr_t[:, off:off + n * esz].bitcast(dt)
        if len(shape) == 3:
            ap = ap.rearrange("p (a b) -> p a b", a=shape[1])
        elif len(shape) == 4:
            ap = ap.rearrange("p (a b c) -> p a b c", a=shape[1], b=shape[2])
        parents = [b_ for (o, e_, b_) in allocs if o < off + nbytes and off < e_]
        b = Buf(ap, parents)
        b.off = off
        allocs.append((off, off + nbytes, b))
        return b

    bank_ctr = [0]
    nbanks = [8]

    def nb():
        b = bank_ctr[0] % nbanks[0]
        bank_ctr[0] += 1
        return b

    def npair():
        if bank_ctr[0] % 2:
            bank_ctr[0] += 1
        b = bank_ctr[0] % 8
        bank_ctr[0] += 2
        return b

    def PB(b):
        return PS.r(b)

    rr = {"cp": 0}

    def copy_op(out_ap, in_ap, reads, writes):
        rr["cp"] ^= 1
        if rr["cp"]:
            pg.add("act", lambda e: e.copy(out_ap, in_ap), reads=reads, writes=writes)
        else:
            pg.add("dve", lambda e: e.tensor_copy(out_ap, in_ap), reads=reads, writes=writes)

    def load(q, out_ap, in_ap, writes):
        return pg.add(q, lambda e: e.dma_start(out=out_ap, in_=in_ap), writes=writes, dma=True)

    def tap(name, ap, shape, dt, regs):
        if name not in tapset:
            return
        d = dram("tap_" + name, shape, dt, out=True)
        op = pg.add("sp", lambda e: e.dma_start(out=d, in_=ap), reads=regs, dma=True)
        pg.final_dmas.append(op)
        tap_out[name] = "tap_" + name

    def xregs0():
        return xT.rs([(0, t) for t in range(5)])

    load("sp", ident.ap, ident_d, [ident.r()])
    load("pool", identb.ap, ident_d, [identb.r()])
    load("sp", cv.ap, cvec_d, [cv.r()])
    load("sp", bada.ap, bada_d, [bada.r()])
    load("sp", nrm.ap, nrm_d, [nrm.r()])
    load("sp", wconv.ap, wconv_d, [wconv.r()])
    load("sp", brs.ap, br_d, [brs.r()])
    pg.add("dve", MEMSET(onesb.ap, 1.0), writes=[onesb.r()])
    load("pool", Ub.ap, U_d, [Ub.r()])
    load("sp", tau.ap, tau_d, [tau.r()])
    load("sp", pconst.ap, pconst_d, [pconst.r()])
    pg.add("act", ACT(scv.ap, cv.ap, AF.Silu), reads=[cv.r()], writes=[scv.r()])

    reset_scratch()
    xs = [carve([128, D], F32) for _ in range(2)]
    wa = [carve([128, 8, 512], BF16) for _ in range(2)]
    mrow = [carve([2, 512], F32) for _ in range(2)]
    for i in range(NT // 128):
        xb = xs[i % 2]
        load("sp", xb.ap, xin[i * 128:(i + 1) * 128, :], [xb.r()])
        t = i // 4
        for half in range(2):
            b = nb()
            fns = [TR(ps[:, b, c4 * 128:(c4 + 1) * 128], xb.ap[:, (half * 4 + c4) * 128:(half * 4 + c4 + 1) * 128], ident.ap) for c4 in range(4)]
            pg.add("pe", seq(fns), reads=[xb.r(), ident.r()], writes=[PB(b)])
            copy_op(xT.ap[:, half * 4:(half + 1) * 4, i * 128:(i + 1) * 128], ps[:, b, :].rearrange("p (a b) -> p a b", a=4),
                    [PB(b)], xT.rs([(c, t) for c in range(half * 4, half * 4 + 4)]))

    zt = carve([128, 8192], BF16)
    HsInit = Buf(Hs_ap)
    RInit = Buf(R_ap)
    HsPrev = [HsInit]
    RPrev = [RInit]
    pg.add("dve", MEMSET(zt.ap, 0.0), writes=[zt.r()])
    for r_ in range(0, NSLOT, 1024):
        n_ = min(1024, NSLOT - r_)
        pg.add("sp", lambda e, o=Hs_ap[r_:r_ + n_, :].rearrange("(p a) n -> p (a n)", a=n_ // 128), i_=zt.ap[:, 0:(n_ // 128) * 1024]: e.dma_start(out=o, in_=i_),
               reads=[zt.r()], writes=[HsInit.r(r_)], dma=True)
    scvb = Buf(sbt("scvb", [128, 16], BF16)[:])
    pg.add("act", ACT(scvb.ap, cv.ap, AF.Silu), reads=[cv.r()], writes=[scvb.r()])

    def compute_mod(li, wab):
        for jb in range(12):
            wb = wab[jb % len(wab)]
            load("pool", wb.ap, w_ada[li, :, jb * 512:(jb + 1) * 512].rearrange("(kc p) n -> p kc n", p=128), [wb.r()])
            b = nb()
            fns = [MM(ps[0:2, b, :], scvb.ap[:, kc * 2:kc * 2 + 2], wb.ap[:, kc, :], start=(kc == 0), stop=(kc == 7)) for kc in range(8)]
            pg.add("pe", seq(fns), reads=[wb.r(), scvb.r()], writes=[PB(b)])
            mr = mrow[jb % 2]
            pg.add("act", lambda e, o=mr.ap[0:2, :], i_=ps[0:2, b, :]: e.copy(o, i_), reads=[PB(b)], writes=[mr.r()])
            b2 = nb()
            fns = [TR(ps[:, b2, j4 * 2:j4 * 2 + 2], mr.ap[0:2, j4 * 128:(j4 + 1) * 128], ident.ap[0:2, 0:2]) for j4 in range(4)]
            pg.add("pe", seq(fns), reads=[mr.r(), ident.r()], writes=[PB(b2)])
            pg.add("dve", TT(mod.ap[:, li, jb * 4:(jb + 1) * 4, :], ps[:, b2, 0:8].rearrange("p (a b) -> p a b", a=4),
                             bada.ap[:, li * 48 + jb * 4:li * 48 + jb * 4 + 4].unsqueeze(2).to_broadcast([128, 4, 2]), ALU.add),
                   reads=[PB(b2), bada.r()], writes=[mod.r(li)])
        for which in range(2):
            base = 8 if which == 0 else 32
            pg.add("dve", STT(gm.ap[:, li, which, :, :], mod.ap[:, li, base:base + 8, :], 1.0,
                              nrm.ap[:, li * 16 + which * 8:li * 16 + which * 8 + 8].unsqueeze(2).to_broadcast([128, 8, 2]),
                              ALU.add, ALU.mult), reads=[mod.r(li), nrm.r()], writes=[gm.r(li)])

    for li_ in range(n_layers):
        compute_mod(li_, wa)
    tap("mod", mod.ap.rearrange("p a b c -> p (a b c)"), [128, 192], F32, [mod.r(0)])
    tap("x0", xT.ap[:, 0, :], [128, NT], F32, xregs0())

    MSH1, MG1, MSH2, MG2 = 0, 16, 24, 40

    NBUF = {}

    def carve_norm():
        NBUF["sq"] = [carve([128, 8, 512], BF16) for _ in range(2)]
        NBUF["tmp"] = [carve([128, 512], F32) for _ in range(4)]
        NBUF["h32"] = [carve([128, 512], F32) for _ in range(4)]
        NBUF["rstd"] = [carve([128, 512], F32) for _ in range(2)]

    def rms_rstd(t, t0, tw):
        b = nb()
        sq = NBUF["sq"][t % 2]
        for hf in range(2):
            pg.add("act", ACT(sq.ap[:, hf * 4:(hf + 1) * 4, 0:tw], xT.ap[:, hf * 4:(hf + 1) * 4, t0:t0 + tw], AF.Square),
                   reads=xT.rs([(c, t) for c in range(hf * 4, hf * 4 + 4)]), writes=[sq.r(hf)])
        pg.add("pe", seq([MM(ps[:, b, 0:tw], onesb.ap, sq.ap[:, c, 0:tw], start=(c == 0), stop=(c == 7)) for c in range(8)]),
               reads=[sq.r(0), sq.r(1), onesb.r()], writes=[PB(b)])
        rs_ = NBUF["rstd"][t % 2]
        pg.add("act", ACT(rs_.ap[:, 0:tw], ps[:, b, 0:tw], AF.Sqrt, scale=1.0 / D, bias=EPS), reads=[PB(b)], writes=[rs_.r()])
        pg.add("dve", RCP(rs_.ap[:, 0:tw], rs_.ap[:, 0:tw]), reads=[rs_.r()], writes=[rs_.r()])
        return rs_

    def norm_phase(li, which, tiles, router=None):
        shb = MSH1 if which == 0 else MSH2
        for (t, (t0, tw)) in tiles:
            s = 0 if t < 4 else 1
            rs_ = rms_rstd(t, t0, tw)
            nch = tw // 128
            rb = [nb() for _ in range(nch)] if router is not None else []
            for c in range(8):
                tm = NBUF["tmp"][c % 4]
                pg.add("dve", STT(tm.ap[:, 0:tw], xT.ap[:, c, t0:t0 + tw], gm.ap[:, li, which, c, s:s + 1], rs_.ap[:, 0:tw], ALU.mult, ALU.mult),
                       reads=[xT.r((c, t)), gm.r(li), rs_.r()], writes=[tm.r()])
                sh_ap = mod.ap[:, li, shb + c, s:s + 1]
                if router is None:
                    pg.add("act", ACT(hT.ap[:, c, t0:t0 + tw], tm.ap[:, 0:tw], AF.Identity, bias=sh_ap, scale=1.0),
                           reads=[tm.r(), mod.r(li)], writes=[hT.r((c, t))])
                else:
                    wr_sb, lg = router
                    hh = NBUF["h32"][c % 4]
                    pg.add("dve", TS(hh.ap[:, 0:tw], tm.ap[:, 0:tw], sh_ap, ALU.add), reads=[tm.r(), mod.r(li)], writes=[hh.r()])
                    pg.add("act", lambda e, o=hT.ap[:, c, t0:t0 + tw], i_=hh.ap[:, 0:tw]: e.copy(o, i_), reads=[hh.r()], writes=[hT.r((c, t))])
                    for i in range(nch):
                        pg.add("pe", MM(ps[:, rb[i], 0:36], hh.ap[:, i * 128:(i + 1) * 128], wr_sb.ap[:, c, :], start=(c == 0), stop=(c == 7)),
                               reads=[hh.r(), wr_sb.r()], writes=[PB(rb[i])])
            if router is not None:
                wr_sb, lg = router
                for i in range(nch):
                    pg.add("dve", TT(lg.ap[:, t * 4 + i, :], ps[:, rb[i], 0:36], brs.ap[:, li * 36:(li + 1) * 36], ALU.add),
                           reads=[PB(rb[i]), brs.r()], writes=[lg.r()])

    def outproj_partial(li, wo, nk, src_fn, src_regs, tiles):
        for (t, (t0, tw)) in tiles:
            s = 0 if t < 4 else 1
            for j in range(8):
                b = nb()
                fns = [MM(ps[:, b, 0:tw], wo.ap[:, k, j * 128:(j + 1) * 128], src_fn(k, t0, tw), start=(k == 0), stop=(k == nk - 1)) for k in range(nk)]
                pg.add("pe", seq(fns), reads=[wo.r()] + src_regs(t), writes=[PB(b)])
                pg.add("dve", STT(xT.ap[:, j, t0:t0 + tw], ps[:, b, 0:tw], mod.ap[:, li, MG1 + j, s:s + 1], xT.ap[:, j, t0:t0 + tw], ALU.mult, ALU.add),
                       reads=[PB(b), mod.r(li), xT.r((j, t))], writes=[xT.r((j, t))])

    def inproj_T(wblk_fn, wreg, tiles, evac):
        for (t, (t0, tw)) in tiles:
            b = nb()
            fns = [MM(ps[:, b, 0:tw], wblk_fn(kc), hT.ap[:, kc, t0:t0 + tw], start=(kc == 0), stop=(kc == 7)) for kc in range(8)]
            pg.add("pe", seq(fns), reads=[wreg] + hT.rs([(kc, t) for kc in range(8)]), writes=[PB(b)])
            evac(t, t0, tw, b)

    for li in range(n_layers):
        last = (li == n_layers - 1)
        tiles_all = list(enumerate(TILES))
        tiles_x = tiles_all[:4]
        tiles_res = tiles_x if last else tiles_all
        nchunks_res = 16 if last else 18

        reset_scratch()
        carve_norm()
        norm_phase(li, 0, tiles_all)
        if li == 0:
            tap("h1", hT.ap[:, 0, :], [128, NT], BF16, hT.rs([(0, t) for t in range(5)]))

        if do_mixer:
            reset_scratch()
            wfb = carve([128, 8, 256], BF16)
            fT = carve([128, 2, NT], BF16)
            wf_sb = carve([128, 2, 256], F32)
            bcs_sb = carve([128, 2, 512], F32)
            WCS = carve([128, 2, 512], BF16)
            PQ = carve([128, 18, 512], BF16)
            tabs = [[carve([128, 16, 256], BF16) for _ in range(2)] for _ in range(2)]
            wo_f = carve([128, 2, D], BF16)
            c2 = carve([128, 2, 256], BF16)
            s2 = carve([128, 2, 256], BF16)
            FwT = carve([128, 2, NT], BF16, at=fT.off)
            load("pool", wfb.ap, w_in[li, :, 0:256].rearrange("(kc p) n -> p kc n", p=128), [wfb.r()])
            load("sp", wf_sb.ap, wf_d[li].rearrange("(kc p) n -> p kc n", p=128), [wf_sb.r()])
            load("sp", bcs_sb.ap, BCS_d.rearrange("(kc p) n -> p kc n", p=128), [bcs_sb.r()])
            load("pool", wo_f.ap, w_out[li, 0:256, :].rearrange("(kc p) n -> p kc n", p=128), [wo_f.r()])
            for mi in range(2):
                b = nb()
                fns = []
                for cs_ in range(2):
                    for kc in range(2):
                        fns.append(MM(ps[:, b, cs_ * 256:(cs_ + 1) * 256], bcs_sb.ap[:, kc, cs_ * 256 + mi * 128:cs_ * 256 + (mi + 1) * 128],
                                      wf_sb.ap[:, kc, :], start=(kc == 0), stop=(kc == 1)))
                pg.add("pe", seq(fns), reads=[bcs_sb.r(), wf_sb.r()], writes=[PB(b)])
                copy_op(WCS.ap[:, mi, :], ps[:, b, :], [PB(b)], [WCS.r()])
            for fc in range(2):
                def evac_f(t, t0, tw, b, fc=fc):
                    copy_op(fT.ap[:, fc, t0:t0 + tw], ps[:, b, 0:tw], [PB(b)], [fT.r((fc, t))])
                inproj_T((lambda fc: lambda kc: wfb.ap[:, kc, fc * 128:(fc + 1) * 128])(fc), wfb.r(), tiles_res, evac_f)
            for i in range(nchunks_res):
                b = nb()
                fns = [MM(ps[:, b, :], fT.ap[:, fc, i * 128:(i + 1) * 128], WCS.ap[:, fc, :], start=(fc == 0), stop=(fc == 1)) for fc in range(2)]
                pg.add("pe", seq(fns), reads=[WCS.r()] + fT.rs([(0, i // 4), (1, i // 4)]), writes=[PB(b)])
                copy_op(PQ.ap[:, i, :], ps[:, b, :], [PB(b)], [PQ.r(i)])
            for kt in range(8):
                tb = tabs[kt % 2]
                load("sp", tb[0].ap, CN_d[:, kt * 256:(kt + 1) * 256].rearrange("(nc p) k -> p nc k", p=128), [tb[0].r()])
                load("sp", tb[1].ap, SN_d[:, kt * 256:(kt + 1) * 256].rearrange("(nc p) k -> p nc k", p=128), [tb[1].r()])
                for fc in range(2):
                    b = nb()
                    fns = []
                    for n_ in range(16):
                        for cs_ in range(2):
                            fns.append(MM(ps[:, b, 0:256], PQ.ap[:, n_, cs_ * 256 + fc * 128:cs_ * 256 + (fc + 1) * 128], tb[cs_].ap[:, n_, :],
                                          start=(n_ == 0 and cs_ == 0), stop=(n_ == 15 and cs_ == 1)))
                    pg.add("pe", seq(fns), reads=[tb[0].r(), tb[1].r()] + PQ.rs(range(16)), writes=[PB(b)])
                    copy_op(FwT.ap[:, fc, kt * 256:(kt + 1) * 256], ps[:, b, 0:256], [PB(b)], [FwT.r(kt // 2)])
            if not last:
                load("sp", c2.ap, C2_d.rearrange("(nc p) k -> p nc k", p=128), [c2.r()])
                load("sp", s2.ap, S2_d.rearrange("(nc p) k -> p nc k", p=128), [s2.r()])
                for fc in range(2):
                    b = nb()
                    fns = []
                    for n_ in range(2):
                        for cs_, tb_ in ((0, c2), (1, s2)):
                            fns.append(MM(ps[:, b, 0:256], PQ.ap[:, 16 + n_, cs_ * 256 + fc * 128:cs_ * 256 + (fc + 1) * 128], tb_.ap[:, n_, :],
                                          start=(n_ == 0 and cs_ == 0), stop=(n_ == 1 and cs_ == 1)))
                    pg.add("pe", seq(fns), reads=[c2.r(), s2.r()] + PQ.rs([16, 17]), writes=[PB(b)])
                    copy_op(FwT.ap[:, fc, 2048:2304], ps[:, b, 0:256], [PB(b)], [FwT.r(4)])
            if li == 0:
                tap("FwT", FwT.ap[:, 0, :], [128, NT], BF16, FwT.rs(range(5)))
            outproj_partial(li, wo_f, 2, lambda k, t0, tw: FwT.ap[:, k, t0:t0 + tw], lambda t: [FwT.r(t)], tiles_res)
            if li == 0:
                tap("xF", xT.ap[:, 0, :], [128, NT], F32, xregs0())

            reset_scratch()
            wg = carve([128, 8, 768], BF16)
            zT = carve([128, 2, NT], BF16)
            gbT = carve([128, 2, NT], BF16)
            convT = carve([128, 2, NT], BF16)
            cacc = carve([128, NT], F32)
            gtmp = [carve([128, 512], F32) for _ in range(2)]
            wo_c = carve([128, 2, D], BF16)
            load("pool", wg.ap, w_in[li, :, 1792:2560].rearrange("(kc p) n -> p kc n", p=128), [wg.r()])
            load("pool", wo_c.ap, w_out[li, 768:1024, :].rearrange("(kc p) n -> p kc n", p=128), [wo_c.r()])
            for cc in range(2):
                def evac_gb(t, t0, tw, b, cc=cc):
                    copy_op(gbT.ap[:, cc, t0:t0 + tw], ps[:, b, 0:tw], [PB(b)], [gbT.r((cc, t))])
                inproj_T((lambda cc: lambda kc: wg.ap[:, kc, cc * 128:(cc + 1) * 128])(cc), wg.r(), tiles_res, evac_gb)

                def evac_gc(t, t0, tw, b, cc=cc):
                    g = gtmp[t % 2]
                    pg.add("act", lambda e, o=g.ap[:, 0:tw], i_=ps[:, b, 0:tw]: e.copy(o, i_), reads=[PB(b)], writes=[g.r()])
                    b2 = nb()
                    fns = [MM(ps[:, b2, 0:tw], wg.ap[:, kc, 512 + cc * 128:512 + (cc + 1) * 128], hT.ap[:, kc, t0:t0 + tw],
                              start=(kc == 0), stop=(kc == 7)) for kc in range(8)]
                    pg.add("pe", seq(fns), reads=[wg.r()] + hT.rs([(kc, t) for kc in range(8)]), writes=[PB(b2)])
                    pg.add("dve", TT(zT.ap[:, cc, t0:t0 + tw], g.ap[:, 0:tw], ps[:, b2, 0:tw], ALU.mult), reads=[g.r(), PB(b2)], writes=[zT.r((cc, t))])
                inproj_T((lambda cc: lambda kc: wg.ap[:, kc, 256 + cc * 128:256 + (cc + 1) * 128])(cc), wg.r(), tiles_res, evac_gc)
                segs = [(0, NX)] if last else [(0, NX), (NX, NT)]
                zregs = zT.rs([(cc, t) for (t, _) in tiles_res])
                wbase = li * 6 + cc * 3
                w0 = wconv.ap[:, wbase + 0:wbase + 1]
                w1 = wconv.ap[:, wbase + 1:wbase + 2]
                w2 = wconv.ap[:, wbase + 2:wbase + 3]
                for (a0, a1) in segs:
                    pg.add("dve", TS(cacc.ap[:, a0:a1], zT.ap[:, cc, a0:a1], w1, ALU.mult), reads=zregs + [wconv.r()], writes=[cacc.r()])
                    pg.add("dve", STT(cacc.ap[:, a0 + 1:a1], zT.ap[:, cc, a0:a1 - 1], w0, cacc.ap[:, a0 + 1:a1], ALU.mult, ALU.add),
                           reads=zregs + [wconv.r(), cacc.r()], writes=[cacc.r()])
                    pg.add("dve", STT(cacc.ap[:, a0:a1 - 1], zT.ap[:, cc, a0 + 1:a1], w2, cacc.ap[:, a0:a1 - 1], ALU.mult, ALU.add),
                           reads=zregs + [wconv.r(), cacc.r()], writes=[cacc.r()])
                    pg.add("dve", TT(convT.ap[:, cc, a0:a1], cacc.ap[:, a0:a1], gbT.ap[:, cc, a0:a1], ALU.mult),
                           reads=[cacc.r()] + gbT.rs([(cc, t) for (t, _) in tiles_res]), writes=[convT.r(cc)])
            if li == 0:
                tap("convT", convT.ap[:, 0, :], [128, NT], BF16, convT.rs([0, 1]))
            outproj_partial(li, wo_c, 2, lambda k, t0, tw: convT.ap[:, k, t0:t0 + tw], lambda t: convT.rs([0, 1]), tiles_res)
            if li == 0:
                tap("xC", xT.ap[:, 0, :], [128, NT], F32, xregs0())

            reset_scratch()
            qT = carve([128, NT], BF16)
            kTm = [carve([128, NT], BF16) for _ in range(2)]
            Va = carve([128, 18, 2, 65], BF16)
            attn = carve([128, 18, 512], BF16)
            biasb = carve([128, 2, 5, 640], BF16)
            tmpS = [carve([128, 640], F32) for _ in range(2)]
            PT = [carve([128, 7, 128], BF16) for _ in range(2)]
            attnT = carve([128, 4, 512], BF16)
            wo_a = carve([128, 4, D], BF16)
            wqkv = [carve([128, 3, 8, 128], BF16) for _ in range(2)]
            rec = [carve([128, 2], F32) for _ in range(2)]
            load("pool", wo_a.ap, w_out[li, 256:768, :].rearrange("(kc p) n -> p kc n", p=128), [wo_a.r()])
            pg.add("dve", MEMSET(Va.ap[:, :, :, 64:65], 1.0), writes=[Va.r("ones")])
            pg.add("dve", MEMSET(kTm[0].ap[64:128, :], 0.0), writes=[kTm[0].r("z")])
            pg.add("dve", MEMSET(kTm[1].ap[0:64, :], 0.0), writes=[kTm[1].r("z")])
            nqb = 16 if last else 18
            for hp in range(4):
                wq = wqkv[hp % 2]
                for m, col0 in enumerate((256, 768, 1280)):
                    load("pool", wq.ap[:, m, :, :], w_in[li, :, col0 + hp * 128:col0 + (hp + 1) * 128].rearrange("(kc p) n -> p kc n", p=128), [wq.r(m)])

                def evac_q(t, t0, tw, b):
                    pg.add("act", ACT(qT.ap[:, t0:t0 + tw], ps[:, b, 0:tw], AF.Identity, scale=0.125), reads=[PB(b)], writes=[qT.r(t)])

                def evac_k(t, t0, tw, b):
                    pg.add("dve", lambda e, o=kTm[0].ap[0:64, t0:t0 + tw], i_=ps[0:64, b, 0:tw]: e.tensor_copy(o, i_), reads=[PB(b)], writes=[kTm[0].r(t)])
                    pg.add("act", lambda e, o=kTm[1].ap[64:128, t0:t0 + tw], i_=ps[64:128, b, 0:tw]: e.copy(o, i_), reads=[PB(b)], writes=[kTm[1].r(t)])
                inproj_T((lambda wq: lambda kc: wq.ap[:, 0, kc, :])(wq), wq.r(0), tiles_res, evac_q)
                inproj_T((lambda wq: lambda kc: wq.ap[:, 1, kc, :])(wq), wq.r(1), tiles_all, evac_k)
                for i4 in range(0, 18, 4):
                    b = nb()
                    n4 = min(4, 18 - i4)
                    fns = []
                    for ii in range(n4):
                        i = i4 + ii
                        for kc in range(8):
                            fns.append(MM(ps[:, b, ii * 128:(ii + 1) * 128], hT.ap[:, kc, i * 128:(i + 1) * 128], wq.ap[:, 2, kc, :],
                                          start=(kc == 0), stop=(kc == 7)))
                    pg.add("pe", seq(fns), reads=[wq.r(2)] + hT.rs([(kc, i4 // 4) for kc in range(8)]), writes=[PB(b)])
                    copy_op(Va.ap[:, i4:i4 + n4, :, 0:64], ps[:, b, 0:n4 * 128].rearrange("p (a h d) -> p a h d", a=n4, h=2),
                            [PB(b)], [Va.r(i4 // 4)])
                vregs_all = [Va.r("ones")] + Va.rs(range(5))
                for hh in range(2):
                    load("pool", biasb.ap[:, hh, :, :].rearrange("p a b -> p (a b)"), biasT_d[li, hp * 2 + hh], [biasb.r(hh)])
                for j in range(nqb):
                    isx = j < 16
                    kchunks = ([cs_of(j) + i for i in range(5)] + [16, 17]) if isx else [16, 17]
                    nk = len(kchunks)
                    ptr = []
                    for hh in range(2):
                        bp = npair()
                        fns = []
                        for ki, kc_ in enumerate(kchunks):
                            o_ = ps[:, bp + ki // 4, (ki % 4) * 128:(ki % 4 + 1) * 128]
                            if isx and ki < 5:
                                fns.append(MM(o_, kTm[hh].ap[:, kc_ * 128:(kc_ + 1) * 128], qT.ap[:, j * 128:(j + 1) * 128], start=True, stop=False))
                                fns.append(MM(o_, identb.ap, biasb.ap[:, hh, cls_of(j), ki * 128:(ki + 1) * 128], start=False, stop=True))
                            else:
                                fns.append(MM(o_, kTm[hh].ap[:, kc_ * 128:(kc_ + 1) * 128], qT.ap[:, j * 128:(j + 1) * 128]))
                        pg.add("pe", seq(fns), reads=[qT.r(j // 4), kTm[hh].r("z"), identb.r(), biasb.r(hh)] + kTm[hh].rs(sorted(set(k_ // 4 for k_ in kchunks))),
                               writes=[PB(bp), PB(bp + 1)])
                        pt = PT[hh]
                        sview = ps[:, bp:bp + 2, :].rearrange("p a b -> p (a b)")
                        if isx:
                            pg.add("act", ACT(pt.ap.rearrange("p a b -> p (a b)"), sview[:, 0:896], AF.Exp),
                                   reads=[PB(bp), PB(bp + 1)], writes=[pt.r("l"), pt.r("c")])
                            ptr.append([pt.r("l"), pt.r("c")])
                        else:
                            pg.add("act", ACT(pt.ap[:, 0:2, :].rearrange("p a b -> p (a b)"), sview[:, 0:256], AF.Exp),
                                   reads=[PB(bp), PB(bp + 1)], writes=[pt.r("l")])
                            ptr.append([pt.r("l")])
                    bo = nb()
                    for hh in range(2):
                        fns = [MM(ps[:, bo, hh * 65:(hh + 1) * 65], PT[hh].ap[:, ki, :], Va.ap[:, kc_, hh, :], start=(ki == 0), stop=(ki == nk - 1))
                               for ki, kc_ in enumerate(kchunks)]
                        pg.add("pe", seq(fns), reads=ptr[hh] + vregs_all, writes=[PB(bo)])
                    rc = rec[j % 2]
                    pv3 = ps[:, bo, 0:130].rearrange("p (h d) -> p h d", h=2)
                    pg.add("dve", RCP(rc.ap[:, 0:2].unsqueeze(2), pv3[:, :, 64:65]), reads=[PB(bo)], writes=[rc.r()])
                    pg.add("dve", TT(attn.ap[:, j, hp * 128:(hp + 1) * 128].rearrange("p (h d) -> p h d", h=2), pv3[:, :, 0:64],
                                     rc.ap[:, 0:2].unsqueeze(2).to_broadcast([128, 2, 64]), ALU.mult),
                           reads=[PB(bo), rc.r()], writes=[attn.r((j // 4, hp * 2)), attn.r((j // 4, hp * 2 + 1))])
            if li == 0:
                tap("attn", attn.ap.rearrange("p a b -> p (a b)"), [128, 18 * 512], BF16, attn.allregs())
            for (t, (t0, tw)) in tiles_res:
                nch = tw // 128
                for a in range(4):
                    b = nb()
                    pbv = ps[:, b, :].bitcast(BF16)
                    fns = [TR(pbv[:, ii * 128:(ii + 1) * 128], attn.ap[:, t * 4 + ii, a * 128:(a + 1) * 128], identb.ap) for ii in range(nch)]
                    pg.add("pe", seq(fns), reads=[identb.r()] + attn.rs([(t, 2 * a), (t, 2 * a + 1)]), writes=[PB(b)])
                    copy_op(attnT.ap[:, a, 0:tw], pbv[:, 0:tw], [PB(b)], [attnT.r(a)])
                outproj_partial(li, wo_a, 4, lambda k, t0, tw: attnT.ap[:, k, 0:tw], lambda t: attnT.rs(range(4)), [(t, (t0, tw))])
            if li == 0:
                tap("xM", xT.ap[:, 0, :], [128, NT], F32, xregs0())

        reset_scratch()
        n_ = nchunks_res
        NTL = NTILES[0] if not last else NTILES[1]
        lg = carve([128, 18, 36], F32)
        wr_sb = carve([128, 8, 36], F32)
        r_gmax = carve([128, 18], F32)
        r_goh = carve([128, 18, 4], F32)
        r_gex = carve([128, 18, 4], F32)
        r_gw = carve([128, 18], F32)
        r_em = carve([128, 18, 4, 8], F32)
        r_es = carve([128, 18, 8], F32)
        r_m1 = carve([128, 18], F32)
        r_o1 = carve([128, 18, 8], F32)
        r_e2 = carve([128, 18, 8], F32)
        r_m2 = carve([128, 18], F32)
        r_o2 = carve([128, 18, 8], F32)
        r_d = carve([128, 18], F32)
        r_w1 = carve([128, 18], F32)
        A1 = carve([128, 18, 4, 8], F32)
        A2 = carve([128, 18, 4, 8], F32)
        Mb = carve([128, 18, 32], BF16)
        rank = carve([128, 18, 32], F32)
        tmpk = carve([128, 18, 32], F32)
        cnt = carve([128, 32], F32)
        ntl = carve([128, 32], F32)
        pa = carve([128, 32], F32)
        pb_ = carve([128, 32], F32)
        segst = carve([128, 32], F32)
        posf = [carve([128, 18], F32) for _ in range(2)]
        cmpb = carve([128, NTMAX, 32], F32)
        etl = carve([128, NTMAX], F32)
        htm = [carve([128, D], BF16) for _ in range(2)]

        carve_norm()
        load("sp", wr_sb.ap, wr_d[li].rearrange("(kc p) n -> p kc n", p=128), [wr_sb.r()])
        norm_phase(li, 1, tiles_res, router=(wr_sb, lg))

        def V(b_):
            return b_.ap[:, 0:n_]
        G = lg.ap[:, 0:n_, 0:4]
        E4 = lg.ap[:, 0:n_, 4:36].rearrange("p n (g e) -> p n g e", g=4)

        def dv(fn, reads, writes):
            pg.add("dve", fn, reads=[x_.r() for x_ in reads], writes=[x_.r() for x_ in writes])

        def bc3(b_, k):
            return V(b_).unsqueeze(2).to_broadcast([128, n_, k])

        pw = [pw1[li], pw2[li]]
        dv(RED(V(r_gmax), G, ALU.max), [lg], [r_gmax])
        dv(TT(V(r_goh), G, bc3(r_gmax, 4), ALU.is_equal), [lg, r_gmax], [r_goh])
        dv(TT(V(r_gex), G, bc3(r_gmax, 4), ALU.subtract), [lg, r_gmax], [r_gex])
        pg.add("act", ACT(V(r_gex), V(r_gex), AF.Exp), reads=[r_gex.r()], writes=[r_gex.r()])
        dv(RED(V(r_gw), V(r_gex), ALU.add), [r_gex], [r_gw])
        dv(RCP(V(r_gw), V(r_gw)), [r_gw], [r_gw])
        dv(TT(V(r_em), E4, V(r_goh).unsqueeze(3).to_broadcast([128, n_, 4, 8]), ALU.mult), [lg, r_goh], [r_em])
        dv(RED(V(r_es), V(r_em).rearrange("p n g e -> p n e g"), ALU.add), [r_em], [r_es])
        dv(RED(V(r_m1), V(r_es), ALU.max), [r_es], [r_m1])
        dv(TT(V(r_o1), V(r_es), bc3(r_m1, 8), ALU.is_equal), [r_es, r_m1], [r_o1])
        dv(STT(V(r_e2), V(r_o1), -1e30, V(r_es), ALU.mult, ALU.add), [r_o1, r_es], [r_e2])
        dv(RED(V(r_m2), V(r_e2), ALU.max), [r_e2], [r_m2])
        dv(TT(V(r_o2), V(r_e2), bc3(r_m2, 8), ALU.is_equal), [r_e2, r_m2], [r_o2])
        dv(TT(V(r_d), V(r_m2), V(r_m1), ALU.subtract), [r_m1, r_m2], [r_d])
        pg.add("act", ACT(V(r_d), V(r_d), AF.Exp), reads=[r_d.r()], writes=[r_d.r()])
        dv(TS(V(r_w1), V(r_d), 1.0, ALU.add), [r_d], [r_w1])
        dv(RCP(V(r_w1), V(r_w1)), [r_w1], [r_w1])
        dv(TT(V(pw[1]), V(r_d), V(r_w1), ALU.mult), [r_d, r_w1], [pw[1]])
        dv(TT(V(pw[0]), V(r_w1), V(r_gw), ALU.mult), [r_w1, r_gw], [pw[0]])
        dv(TT(V(pw[1]), V(pw[1]), V(r_gw), ALU.mult), [pw[1], r_gw], [pw[1]])
        gohb = V(r_goh).unsqueeze(3).to_broadcast([128, n_, 4, 8])
        dv(TT(V(A1), gohb, V(r_o1).unsqueeze(2).to_broadcast([128, n_, 4, 8]), ALU.mult), [r_goh, r_o1], [A1])
        dv(TT(V(A2), gohb, V(r_o2).unsqueeze(2).to_broadcast([128, n_, 4, 8]), ALU.mult), [r_goh, r_o2], [A2])
        A1f = V(A1).rearrange("p n g e -> p n (g e)")
        A2f = V(A2).rearrange("p n g e -> p n (g e)")
        dv(TT(V(Mb), A1f, A2f, ALU.add), [A1, A2], [Mb])
        if li == 0:
            tap("lg", lg.ap.rearrange("p a b -> p (a b)"), [128, 18 * 36], F32, [lg.r()])
        brk = [nb(), nb()]
        for bi in range(2):
            lo, hi = bi * 16, min(n_, bi * 16 + 16)
            if lo >= hi:
                continue
            fns = []
            for i in range(lo, hi):
                col = (i - lo) * 32
                for i2 in range(i):
                    fns.append(MM(ps[:, brk[bi], col:col + 32], onesb.ap, Mb.ap[:, i2, :], start=(i2 == 0), stop=False))
                fns.append(MM(ps[:, brk[bi], col:col + 32], Ub.ap, Mb.ap[:, i, :], start=(i == 0), stop=True))
            pg.add("pe", seq(fns), reads=[onesb.r(), Ub.r(), Mb.r()], writes=[PB(brk[bi])])
            copy_op(rank.ap[:, lo:hi, :], ps[:, brk[bi], 0:(hi - lo) * 32].rearrange("p (a b) -> p a b", b=32), [PB(brk[bi])], [rank.r(bi)])
        bcn = nb()
        pg.add("pe", seq([MM(ps[:, bcn, 0:32], onesb.ap, Mb.ap[:, i, :], start=(i == 0), stop=(i == n_ - 1)) for i in range(n_)]),
               reads=[onesb.r(), Mb.r()], writes=[PB(bcn)])
        pg.add("dve", lambda e, o=cnt.ap, i_=ps[:, bcn, 0:32]: e.tensor_copy(o, i_), reads=[PB(bcn)], writes=[cnt.r()])
        dv(TS(ntl.ap, cnt.ap, 0.0, ALU.is_gt), [cnt], [ntl])
        for m in range(1, (NT + TW - 1) // TW):
            dv(STT(ntl.ap, cnt.ap, float(TW * m), ntl.ap, ALU.is_gt, ALU.add), [cnt, ntl], [ntl])
        src, dst = ntl, pa
        for sh in (1, 2, 4, 8, 16):
            dv(lambda e, o=dst.ap[:, 0:sh], i_=src.ap[:, 0:sh]: e.tensor_copy(o, i_), [src], [dst])
            dv(TT(dst.ap[:, sh:32], src.ap[:, sh:32], src.ap[:, 0:32 - sh], ALU.add), [src, dst], [dst])
            src, dst = dst, (pb_ if dst is pa else pa)
        incl = src
        dv(TT(segst.ap, incl.ap, ntl.ap, ALU.subtract), [incl, ntl], [segst])
        dv(TS(segst.ap, segst.ap, float(TW), ALU.mult), [segst], [segst])
        pg.add("dve", TT(V(rank), V(rank), segst.ap.unsqueeze(1).to_broadcast([128, n_, 32]), ALU.add),
               reads=[rank.r(0), rank.r(1), segst.r()], writes=[rank.r(0), rank.r(1)])
        for k, Af in ((0, A1f), (1, A2f)):
            pg.add("dve", TT(V(tmpk), V(rank), Af, ALU.mult), reads=rank.allregs() + [A1.r(), A2.r()], writes=[tmpk.r()])
            dv(RED(V(posf[k]), V(tmpk), ALU.add), [tmpk], [posf[k]])
            dv(lambda e, o=V(posi[li][k]), i_=V(posf[k]): e.tensor_copy(o, i_), [posf[k]], [posi[li][k]])
        dv(TT(cmpb.ap[:, 0:NTL, :], incl.ap.unsqueeze(1).to_broadcast([128, NTL, 32]), tau.ap[:, 0:NTL].unsqueeze(2).to_broadcast([128, NTL, 32]), ALU.is_le),
           [incl, tau], [cmpb])
        dv(RED(etl.ap[:, 0:NTL], cmpb.ap[:, 0:NTL, :], ALU.add), [cmpb], [etl])
        dv(TS(etl.ap[:, 0:NTL], etl.ap[:, 0:NTL], 31.0, ALU.min), [etl], [etl])
        dv(lambda e, o=etl.ap[:, 0:NTL], c_=pconst.ap[:, li:li + 1]: e.tensor_scalar(out=o, in0=o, scalar1=128.0, scalar2=c_, op0=ALU.mult, op1=ALU.add),
           [etl, pconst], [etl])
        dv(lambda e, o=widx[li].ap[:, 0:NTL], i_=etl.ap[:, 0:NTL]: e.tensor_copy(o, i_), [etl], [widx[li]])
        if li == 0:
            tap("pos1", posf[0].ap, [128, 18], F32, [posf[0].r()])
            tap("pos2", posf[1].ap, [128, 18], F32, [posf[1].r()])
            tap("etl", etl.ap, [128, NTMAX], F32, [etl.r()])
        HsL = Buf(Hs_ap, parents=[HsPrev[0]])
        for i in range(n_):
            b = nb()
            pbv = ps[:, b, :].bitcast(BF16)
            fns = [TR(pbv[:, kc * 128:(kc + 1) * 128], hT.ap[:, kc, i * 128:(i + 1) * 128], identb.ap) for kc in range(8)]
            pg.add("pe", seq(fns), reads=[identb.r()] + hT.rs([(kc, i // 4) for kc in range(8)]), writes=[PB(b)])
            hb_ = htm[i % 2]
            copy_op(hb_.ap, pbv, [PB(b)], [hb_.r()])
            for k in range(2):
                pg.add("pool", lambda e, i_=hb_.ap, off=posi[li][k].ap[:, i:i + 1]: e.indirect_dma_start(
                    out=Hs_ap, out_offset=bass.IndirectOffsetOnAxis(ap=off, axis=0), in_=i_, in_offset=None),
                    reads=[hb_.r(), posi[li][k].r()], writes=[HsL.r((i, k))], dma=True)
        HsPrev[0] = HsL
        hs_regs = HsL.allregs()

        reset_scratch()
        wgu = [carve([128, 8192], BF16) for _ in range(2)]
        wdn = [carve([128, 4096], BF16) for _ in range(2)]
        hid = [carve([128, 4, TW], BF16) for _ in range(2)]
        sg = [carve([128, TW], BF16) for _ in range(3)]
        rst = [carve([128, D], F32) for _ in range(2)]
        hg = [Buf(hT_t[:, q * 4096:q * 4096 + 8 * TW].rearrange("p (a b) -> p a b", a=8), parents=[hT]) for q in range(2)]
        hst = [Buf(hT_t[:, 8192 + q * 4096:8192 + q * 4096 + NSC * 1024].rearrange("p (a b) -> p a b", a=NSC), parents=[hT]) for q in range(2)]
        RL = Buf(R_ap, parents=[RPrev[0]])

        def load_dn(tt):
            ws = wdn[tt % 2]
            pg.add("pool", lambda e, o=ws.ap, off=widx[li].ap[:, tt:tt + 1]: e.indirect_dma_start(
                out=o, out_offset=None, in_=W_dn, in_offset=bass.IndirectOffsetOnAxis(ap=off, axis=0)),
                reads=[widx[li].r()], writes=[ws.r()], dma=True)

        def tile_loads(tt):
            ws = wgu[tt % 2]
            pg.add("pool", lambda e, o=ws.ap, off=widx[li].ap[:, tt:tt + 1]: e.indirect_dma_start(
                out=o, out_offset=None, in_=W_gu, in_offset=bass.IndirectOffsetOnAxis(ap=off, axis=0)),
                reads=[widx[li].r()], writes=[ws.r()], dma=True)
            hs_ = hst[tt % 2]
            pg.add("sp", lambda e, o=hs_.ap, i_=Hs_ap[tt * TW:(tt + 1) * TW, :].rearrange("(c p) n -> p c n", p=128): e.dma_start(out=o, in_=i_),
                   reads=hs_regs, writes=[hs_.r()], dma=True)

        def tile_front(tt):
            ws = wgu[tt % 2]
            hs_ = hst[tt % 2]
            hg_ = hg[tt % 2]
            hb = hid[tt % 2]
            for kc in range(8):
                b = nb()
                pbv = ps[:, b, :].bitcast(BF16)
                fns = [TR(pbv[:, c * 128:(c + 1) * 128], hs_.ap[:, c, kc * 128:(kc + 1) * 128], identb.ap) for c in range(NSC)]
                pg.add("pe", seq(fns), reads=[identb.r(), hs_.r()], writes=[PB(b)])
                copy_op(hg_.ap[:, kc, :], pbv[:, 0:TW], [PB(b)], [hg_.r(kc)])
            hregs = hg_.rs(range(8))
            wg_ = ws.ap[:, 0:4096].rearrange("p (a b) -> p a b", a=8)
            wu_ = ws.ap[:, 4096:8192].rearrange("p (a b) -> p a b", a=8)
            for dc in range(4):
                bg = nb()
                bu = nb()
                fg = [MM(ps[:, bg, 0:TW], wg_[:, kc, dc * 128:(dc + 1) * 128], hg_.ap[:, kc, :], start=(kc == 0), stop=(kc == 7)) for kc in range(8)]
                fu = [MM(ps[:, bu, 0:TW], wu_[:, kc, dc * 128:(dc + 1) * 128], hg_.ap[:, kc, :], start=(kc == 0), stop=(kc == 7)) for kc in range(8)]
                pg.add("pe", seq(fg), reads=[ws.r()] + hregs, writes=[PB(bg)])
                pg.add("pe", seq(fu), reads=[ws.r()] + hregs, writes=[PB(bu)])
                sgb = sg[dc % 3]
                pg.add("act", ACT(sgb.ap, ps[:, bg, 0:TW], AF.Silu), reads=[PB(bg)], writes=[sgb.r()])
                pg.add("dve", TT(hb.ap[:, dc, :], sgb.ap, ps[:, bu, 0:TW], ALU.mult), reads=[sgb.r(), PB(bu)], writes=[hb.r(dc)])

        def tile_back(tt):
            ws = wdn[tt % 2]
            hb = hid[tt % 2]
            wd_ = ws.ap.rearrange("p (a b) -> p a b", a=4)
            for sc_ in range(NSC):
                ro = rst[(tt * NSC + sc_) % 2]
                for half in range(2):
                    b = nb()
                    fns = [MM(ps[:, b, :], hb.ap[:, dc, sc_ * 128:(sc_ + 1) * 128], wd_[:, dc, half * 512:(half + 1) * 512], start=(dc == 0), stop=(dc == 3))
                           for dc in range(4)]
                    pg.add("pe", seq(fns), reads=[ws.r()] + hb.rs(range(4)), writes=[PB(b)])
                    copy_op(ro.ap[:, half * 512:(half + 1) * 512], ps[:, b, :], [PB(b)], [ro.r(half)])
                r0 = tt * TW + sc_ * 128
                pg.add("sp", lambda e, o=R_ap[r0:r0 + 128, :], i_=ro.ap: e.dma_start(out=o, in_=i_), reads=[ro.r(0), ro.r(1)], writes=[RL.r((tt, sc_))], dma=True)

        tile_loads(0)
        load_dn(0)
        for tt in range(NTL):
            if tt + 1 < NTL:
                tile_loads(tt + 1)
            tile_front(tt)
            if tt > 0:
                tile_back(tt - 1)
            if tt + 1 < NTL:
                load_dn(tt + 1)
        tile_back(NTL - 1)
        RPrev[0] = RL
        r_regs = RL.allregs()

        reset_scratch()
        Gb = [[carve([128, D], F32) for _ in range(2)] for _ in range(2)]
        yb = [carve([128, D], F32) for _ in range(2)]
        for i in range(n_):
            t = i // 4
            s = 0 if t < 4 else 1
            for k in range(2):
                g_ = Gb[k][i % 2]
                pg.add("pool", lambda e, o=g_.ap, off=posi[li][k].ap[:, i:i + 1]: e.indirect_dma_start(
                    out=o, out_offset=None, in_=R_ap, in_offset=bass.IndirectOffsetOnAxis(ap=off, axis=0)),
                    reads=r_regs + [posi[li][k].r()], writes=[g_.r()], dma=True)
            y_ = yb[i % 2]
            pg.add("dve", TS(y_.ap, Gb[0][i % 2].ap, pw[0].ap[:, i:i + 1], ALU.mult), reads=[Gb[0][i % 2].r(), pw[0].r()], writes=[y_.r()])
            pg.add("dve", STT(y_.ap, Gb[1][i % 2].ap, pw[1].ap[:, i:i + 1], y_.ap, ALU.mult, ALU.add), reads=[Gb[1][i % 2].r(), pw[1].r(), y_.r()], writes=[y_.r()])
            for half in range(2):
                b = nb()
                fns = [TR(ps[:, b, c4 * 128:(c4 + 1) * 128], y_.ap[:, (half * 4 + c4) * 128:(half * 4 + c4 + 1) * 128], ident.ap) for c4 in range(4)]
                pg.add("pe", seq(fns), reads=[ident.r(), y_.r()], writes=[PB(b)])
                for c4 in range(4):
                    j = half * 4 + c4
                    pg.add("dve", STT(xT.ap[:, j, i * 128:(i + 1) * 128], ps[:, b, c4 * 128:(c4 + 1) * 128], mod.ap[:, li, MG2 + j, s:s + 1],
                                      xT.ap[:, j, i * 128:(i + 1) * 128], ALU.mult, ALU.add),
                           reads=[PB(b), mod.r(li), xT.r((j, t))], writes=[xT.r((j, t))])
        for g in hT.allregs():
            _inherit(g, [r_ for b_ in hg + hst for r_ in b_.allregs()])
        if li == 0:
            tap("x1", xT.ap[:, 0, :], [128, NT], F32, xregs0())

    reset_scratch()
    yT = carve([128, 8, 512], F32)
    ob = [carve([128, D], F32) for _ in range(2)]
    carve_norm()
    nfb = 32
    for (t, (t0, tw)) in list(enumerate(TILES))[:4]:
        rs_ = rms_rstd(t, t0, tw)
        for c in range(8):
            pg.add("dve", STT(yT.ap[:, c, 0:tw], xT.ap[:, c, t0:t0 + tw], nrm.ap[:, nfb + c:nfb + c + 1], rs_.ap[:, 0:tw], ALU.mult, ALU.mult),
                   reads=[xT.r((c, t)), nrm.r(), rs_.r()], writes=[yT.r(c)])
        for ii in range(4):
            i = t * 4 + ii
            o_ = ob[i % 2]
            for half in range(2):
                b = nb()
                fns = [TR(ps[:, b, c4 * 128:(c4 + 1) * 128], yT.ap[:, half * 4 + c4, ii * 128:(ii + 1) * 128], ident.ap) for c4 in range(4)]
                pg.add("pe", seq(fns), reads=[ident.r()] + yT.rs(range(half * 4, half * 4 + 4)), writes=[PB(b)])
                copy_op(o_.ap[:, half * 512:(half + 1) * 512], ps[:, b, :], [PB(b)], [o_.r(half)])
            op = pg.add("sp", lambda e, o=out_d[i * 128:(i + 1) * 128, :], i_=o_.ap: e.dma_start(out=o, in_=i_), reads=[o_.r(0), o_.r(1)], dma=True)
            pg.final_dmas.append(op)

    pg.emit(st)
    st.close()
    return nc, tap_out


_CONST = {}


def _constants():
    if _CONST:
        return _CONST
    bf = ml_dtypes.bfloat16
    n = np.arange(NX)
    ang = 2.0 * np.pi * ((n[:, None] * n[None, :]) % NX).astype(np.float64) / NX
    _CONST["CN"] = (np.cos(ang) / np.sqrt(NX)).astype(np.float32).astype(bf)
    _CONST["SN"] = (-np.sin(ang) / np.sqrt(NX)).astype(np.float32).astype(bf)
    m = np.arange(256)
    ang2 = 2.0 * np.pi * ((m[:, None] * m[None, :]) % 256).astype(np.float64) / 256
    _CONST["C2"] = (np.cos(ang2) / 16.0).astype(np.float32).astype(bf)
    _CONST["S2"] = (-np.sin(ang2) / 16.0).astype(np.float32).astype(bf)
    c = np.arange(64)
    angc = 2.0 * np.pi * ((c[:, None] * c[None, :]) % 64).astype(np.float64) / 64
    bc = np.zeros((256, 256), np.float32)
    bs = np.zeros((256, 256), np.float32)
    for g in range(4):
        bc[g * 64:(g + 1) * 64, g * 64:(g + 1) * 64] = np.cos(angc) / 8.0
        bs[g * 64:(g + 1) * 64, g * 64:(g + 1) * 64] = np.sin(angc) / 8.0
    _CONST["BCS"] = np.concatenate([bc, bs], axis=1)
    _CONST["ident"] = np.eye(128, dtype=np.float32)
    _CONST["Uc"] = np.triu(np.ones((128, 128), np.float32), k=1)
    _CONST["tau"] = np.ascontiguousarray(np.broadcast_to(np.arange(NTMAX, dtype=np.float32)[None, :], (128, NTMAX)))
    _CONST["pconst"] = np.stack([np.arange(128, dtype=np.float32), np.arange(128, dtype=np.float32) + 4096.0], axis=1)
    ro = np.zeros((5, 128, 5, 128), np.int64)
    co = np.zeros((5, 128, 5, 128), np.int64)
    va = np.zeros((5, 128, 5, 128), bool)
    kk = np.arange(128)
    kr, kcol = kk // 64, kk % 64
    qr, qc = kk // 64, kk % 64
    for cl, j in enumerate((0, 1, 2, 14, 15)):
        r = 2 * j + qr
        rstart = np.clip(r - 4, 0, 24)
        cstart = np.clip(qc - 8, 0, 48)
        for i in range(5):
            krow = 2 * (cs_of(j) + i) + kr
            okr = (krow[:, None] >= rstart[None, :]) & (krow[:, None] < rstart[None, :] + 8)
            okc = (kcol[:, None] >= cstart[None, :]) & (kcol[:, None] < cstart[None, :] + 16)
            va[cl, :, i, :] = okr & okc
            ro[cl, :, i, :] = np.clip(krow[:, None] - r[None, :] + 7, 0, 14)
            co[cl, :, i, :] = np.clip(kcol[:, None] - qc[None, :] + 15, 0, 30)
    _CONST["ro"], _CONST["co"], _CONST["va"] = ro, co, va
    return _CONST


def _fm(v):
    v = np.asarray(v, np.float32)
    return np.ascontiguousarray(v.reshape(-1, 128).T)


def prepare_inputs(x, c, ctx, c_ctx, w_ada, b_ada, norm1, norm2, w_in, w_fourier, w_conv, rpb, w_out,
                   w_rg, b_rg, w_re, b_re, w_gate, w_up, w_down, norm_final):
    K = _constants()
    f32 = lambda a: np.ascontiguousarray(np.asarray(a, np.float32))
    shared = {
        "w_ada": f32(w_ada), "w_in": f32(w_in), "w_out": f32(w_out), "wf": f32(w_fourier),
        "CN": K["CN"], "SN": K["SN"], "C2": K["C2"], "S2": K["S2"], "BCS": K["BCS"], "ident": K["ident"],
    }
    shared["bada"] = np.concatenate([_fm(b_ada[0]), _fm(b_ada[1])], axis=1)
    shared["nrm"] = np.concatenate([_fm(norm1[0]), _fm(norm2[0]), _fm(norm1[1]), _fm(norm2[1]), _fm(norm_final)], axis=1)
    wc = np.asarray(w_conv, np.float32)
    shared["wconv"] = np.ascontiguousarray(wc.reshape(2, 3, 2, 128).transpose(3, 0, 2, 1).reshape(128, 12))
    shared["wr"] = np.ascontiguousarray(np.concatenate([np.asarray(w_rg, np.float32), np.asarray(w_re, np.float32)], axis=2))
    brv = np.concatenate([np.asarray(b_rg, np.float32), np.asarray(b_re, np.float32)], axis=1).reshape(1, 72)
    shared["br"] = np.ascontiguousarray(np.broadcast_to(brv, (128, 72)))
    rp = np.asarray(rpb, np.float32)
    g = rp[:, :, K["ro"], K["co"]]
    g = np.where(K["va"][None, None], g, np.float32(-30000.0))
    shared["biasT"] = np.ascontiguousarray(g.transpose(0, 1, 3, 2, 4, 5).reshape(2, 8, 128, 3200).astype(np.float32))
    wgu = np.empty((2, NE, 128, 8192), np.float32)
    wgu[..., 0:4096] = np.asarray(w_gate, np.float32).reshape(2, NE, 8, 128, 512).transpose(0, 1, 3, 2, 4).reshape(2, NE, 128, 4096)
    wgu[..., 4096:8192] = np.asarray(w_up, np.float32).reshape(2, NE, 8, 128, 512).transpose(0, 1, 3, 2, 4).reshape(2, NE, 128, 4096)
    shared["W_gu"] = wgu.reshape(2 * NE * 128, 8192)
    shared["W_dn"] = np.ascontiguousarray(
        np.asarray(w_down, np.float32).reshape(2, NE, 4, 128, 1024).transpose(0, 1, 3, 2, 4).reshape(2 * NE * 128, 4096))
    shared["Uc"] = K["Uc"]
    shared["tau"] = K["tau"]
    shared["pconst"] = K["pconst"]
    in_maps = []
    for b in range(8):
        m = dict(shared)
        m["xin"] = np.ascontiguousarray(np.concatenate([np.asarray(x[b], np.float32), np.asarray(ctx[b], np.float32)], axis=0))
        cvm = np.stack([_fm(c[b]), _fm(c_ctx)], axis=2).reshape(128, 16)
        m["cvec"] = np.ascontiguousarray(cvm)
        in_maps.append(m)
    return in_maps


_PROG = {}


def kernel(**inputs):
    if "nc" not in _PROG:
        _PROG["nc"] = build_program()[0]
    in_maps = prepare_inputs(**inputs)
    res = run_bass_kernel_spmd(_PROG["nc"], in_maps, core_ids=list(range(8)))
    return np.stack([np.asarray(r["out"], np.float32) for r in res.results], axis=0)
```

```python
import numpy as np
import ml_dtypes
from contextlib import ExitStack
import concourse.bass as bass
import concourse.mybir as mybir
from concourse.bass_utils import run_bass_kernel_spmd

F32 = mybir.dt.float32
BF16 = mybir.dt.bfloat16
U8 = mybir.dt.uint8
I32 = mybir.dt.int32
AF = mybir.ActivationFunctionType
ALU = mybir.AluOpType
AX = mybir.AxisListType

COMPUTE = ("pe", "act", "dve", "pool")
import os
SAME_ENGINE_SYNC = os.environ.get('SES', '1') == '1'
ATT_SKEW = os.environ.get('ATT_SKEW', '0') == '1'


class Reg:
    __slots__ = ("writer", "rd", "rdma")

    def __init__(self):
        self.writer = None
        self.rd = {}
        self.rdma = []


def _inherit(g, parents):
    for p in parents:
        cands = list(p.rd.values())
        if p.writer is not None:
            cands.append(p.writer)
        for op in cands:
            if op.dma:
                g.rdma.append(op)
            else:
                o = g.rd.get(op.eng)
                if o is None or o.pos < op.pos:
                    g.rd[op.eng] = op
        g.rdma.extend(p.rdma)


class Buf:
    def __init__(self, ap, parents=()):
        self.ap = ap
        self.parents = list(parents)
        self.regs = {}

    def r(self, key=0):
        g = self.regs.get(key)
        if g is None:
            g = Reg()
            for pb in self.parents:
                _inherit(g, pb.allregs())
            self.regs[key] = g
        return g

    def rs(self, keys):
        return [self.r(k) for k in keys]

    def allregs(self):
        return list(self.regs.values())


class Op:
    __slots__ = ("eng", "fn", "deps", "dma", "pos", "waits", "signal", "dsem", "dval", "dprev")


class Prog:
    def __init__(self, nc, n_dma_sems=20):
        self.nc = nc
        self.ops = []
        self.streams = {e: [] for e in ("pe", "act", "dve", "pool", "sp")}
        self.n_dma_sems = n_dma_sems
        self.dma_rr = {"sp": 0, "pool": 0, "act": 0}
        self.dma_tot = {}
        self.final_dmas = []

    def add(self, eng, fn, reads=(), writes=(), dma=False):
        op = Op()
        op.eng = eng
        op.fn = fn
        op.dma = dma
        op.waits = []
        op.signal = False
        deps = []
        for r in reads:
            if r.writer is not None:
                deps.append(r.writer)
        for w in writes:
            if w.writer is not None:
                deps.append(w.writer)
            deps.extend(w.rd.values())
            deps.extend(w.rdma)
        for r in reads:
            if dma:
                r.rdma.append(op)
            else:
                r.rd[eng] = op
        for w in writes:
            w.writer = op
            w.rd = {}
            w.rdma = []
        op.deps = deps
        op.pos = len(self.streams[eng])
        self.streams[eng].append(op)
        self.ops.append(op)
        if dma:
            k = self.dma_rr[eng]
            self.dma_rr[eng] = (k + 1) % self.n_dma_sems
            key = (eng, k)
            prev = self.dma_tot.get(key, 0)
            op.dsem = key
            op.dprev = prev
            op.dval = prev + 16
            self.dma_tot[key] = prev + 16
        return op

    def emit(self, stack):
        nc = self.nc
        sems = {e: stack.enter_context(nc.semaphore("s_" + e)) for e in COMPUTE}
        dsems = {}
        for key in self.dma_tot:
            dsems[key] = stack.enter_context(nc.semaphore("d_%s%d" % key))
        waited = {c: {} for c in self.streams}
        for op in self.ops:
            c = op.eng
            w = waited[c]
            best = {}
            for d in op.deps:
                if d is op:
                    continue
                if d.dma:
                    if w.get(d.dsem, 0) < d.dval:
                        w[d.dsem] = d.dval
                        op.waits.append(("d", d.dsem, d.dval))
                else:
                    if d.eng == c and (c == "pe" or not SAME_ENGINE_SYNC):
                        continue
                    if d.pos > best.get(d.eng, -1):
                        best[d.eng] = d.pos
            for e, p in best.items():
                if w.get(e, -1) < p:
                    w[e] = p
                    prod = self.streams[e][p]
                    prod.signal = True
                    op.waits.append(("c", e, prod))
            if op.dma and op.dprev > 0:
                if w.get(op.dsem, 0) < op.dprev:
                    w[op.dsem] = op.dprev
                    op.waits.append(("d", op.dsem, op.dprev))
            op.deps = None
        for e in COMPUTE:
            cnt = 0
            for op in self.streams[e]:
                if op.signal and not op.dma:
                    cnt += 1
                    op.dval = cnt
        final = list(self.final_dmas)

        def run_stream(engname, eng):
            for op in self.streams[engname]:
                for kind, key, v in op.waits:
                    if kind == "d":
                        eng.wait_ge(dsems[key], v)
                    else:
                        eng.wait_ge(sems[key], v.dval)
                ins = op.fn(eng)
                if op.dma:
                    ins.then_inc(dsems[op.dsem], 16)
                elif op.signal:
                    ins.then_inc(sems[engname], 1)
            if engname == "sp":
                for op in final:
                    eng.wait_ge(dsems[op.dsem], op.dval)

        with nc.Block() as block:
            @block.sync
            def _(e):
                run_stream("sp", e)

            @block.tensor
            def _(e):
                run_stream("pe", e)

            @block.scalar
            def _(e):
                run_stream("act", e)

            @block.vector
            def _(e):
                run_stream("dve", e)

            @block.gpsimd
            def _(e):
                run_stream("pool", e)


def seq(fns):
    def f(e):
        ins = None
        for g in fns:
            ins = g(e)
        return ins
    return f


D = 1024
NX = 2048
NC_ = 256
NT = NX + NC_
NE = 32
EPS = 1e-6
TILES = [(0, 512), (512, 512), (1024, 512), (1536, 512), (2048, 256)]
SCR_BYTES = 97400
TW = int(os.environ.get("TW", "256"))
NSC = TW // 128
NTILES = ((4608 + 32 * (TW - 1)) // TW + int(os.environ.get("XT", "0")), (4096 + 32 * (TW - 1)) // TW + int(os.environ.get("XT", "0")))
NTMAX = NTILES[0]
NSLOT = NTMAX * TW


def cs_of(j):
    return min(max(j - 2, 0), 11)


def cls_of(j):
    return {0: 0, 1: 1, 14: 3, 15: 4}.get(j, 2)


def MM(out, lhsT, rhs, start=True, stop=True):
    return lambda e: e.matmul(out, lhsT, rhs, start=start, stop=stop)


def TR(out, in_, idn):
    return lambda e: e.transpose(out, in_, idn)


def ACT(out, in_, func, **kw):
    return lambda e: e.activation(out=out, in_=in_, func=func, **kw)


def TT(out, in0, in1, op):
    return lambda e: e.tensor_tensor(out=out, in0=in0, in1=in1, op=op)


def STT(out, in0, scalar, in1, op0, op1):
    return lambda e: e.scalar_tensor_tensor(out=out, in0=in0, scalar=scalar, in1=in1, op0=op0, op1=op1)


def TS(out, in0, s1, op0):
    return lambda e: e.tensor_scalar(out=out, in0=in0, scalar1=s1, scalar2=None, op0=op0)


def RCP(out, in_):
    return lambda e: e.reciprocal(out=out, in_=in_)


def RED(out, in_, op):
    return lambda e: e.tensor_reduce(out=out, in_=in_, axis=AX.X, op=op)


def MEMSET(ap, v):
    return lambda e: e.memset(ap, v)


def build_program(n_layers=2, taps=(), moe_experts=NE, do_mixer=True):
    nc = bass.Bass("TRN2", target_bir_lowering=False)
    st = ExitStack()
    pg = Prog(nc)
    tapset = set(taps)
    tap_out = {}

    def dram(name, shape, dt=F32, out=False):
        return nc.dram_tensor(name, list(shape), dt, kind="ExternalOutput" if out else "ExternalInput").ap()

    xin = dram("xin", [NT, D])
    cvec_d = dram("cvec", [128, 16])
    w_ada = dram("w_ada", [2, D, 6 * D])
    bada_d = dram("bada", [128, 96])
    nrm_d = dram("nrm", [128, 40])
    w_in = dram("w_in", [2, D, 2560])
    w_out = dram("w_out", [2, D, D])
    wf_d = dram("wf", [2, 256, 256])
    wconv_d = dram("wconv", [128, 12])
    wr_d = dram("wr", [2, D, 36])
    br_d = dram("br", [128, 72])
    W_gu = dram("W_gu", [2 * NE * 128, 8192])
    W_dn = dram("W_dn", [2 * NE * 128, 4096])
    U_d = dram("Uc", [128, 128])
    tau_d = dram("tau", [128, NTMAX])
    pconst_d = dram("pconst", [128, 2])
    Hs_ap = nc.dram_tensor("Hs", [NSLOT, D], BF16, kind="Internal").ap()
    R_ap = nc.dram_tensor("Rr", [NSLOT, D], F32, kind="Internal").ap()
    biasT_d = dram("biasT", [2, 8, 128, 3200])
    CN_d = dram("CN", [NX, NX], BF16)
    SN_d = dram("SN", [NX, NX], BF16)
    C2_d = dram("C2", [256, 256], BF16)
    S2_d = dram("S2", [256, 256], BF16)
    BCS_d = dram("BCS", [256, 512])
    ident_d = dram("ident", [128, 128])
    out_d = dram("out", [NX, D], out=True)

    def sbt(name, shape, dt=F32):
        return st.enter_context(nc.sbuf_tensor(name, list(shape), dt))

    xT = Buf(sbt("xT", [128, 8, NT])[:])
    hT_t = sbt("hT", [128, 8 * NT], BF16)
    hT = Buf(hT_t[:].rearrange("p (c t) -> p c t", c=8))
    ident = Buf(sbt("ident_sb", [128, 128])[:])
    identb = Buf(sbt("identb", [128, 128], BF16)[:])
    onesb = Buf(sbt("onesb", [128, 128], BF16)[:])
    cv = Buf(sbt("cv", [128, 16])[:])
    scv = Buf(sbt("scv", [128, 16])[:])
    bada = Buf(sbt("bada_sb", [128, 96])[:])
    nrm = Buf(sbt("nrm_sb", [128, 40])[:])
    wconv = Buf(sbt("wconv_sb", [128, 12])[:])
    brs = Buf(sbt("br_sb", [128, 72])[:])
    mod = Buf(sbt("mod", [128, 2, 48, 2])[:])
    gm = Buf(sbt("gm", [128, 2, 2, 8, 2])[:])
    Ub = Buf(sbt("Ub", [128, 128], BF16)[:])
    tau = Buf(sbt("tau_sb", [128, NTMAX])[:])
    pconst = Buf(sbt("pconst_sb", [128, 2])[:])
    pw1 = [Buf(sbt("pw1_%d" % l_, [128, 18])[:]) for l_ in range(2)]
    pw2 = [Buf(sbt("pw2_%d" % l_, [128, 18])[:]) for l_ in range(2)]
    posi = [[Buf(sbt("posi%d_%d" % (l_, k_), [128, 18], I32)[:]) for k_ in range(2)] for l_ in range(2)]
    widx = [Buf(sbt("widx%d" % l_, [128, NTMAX], I32)[:]) for l_ in range(2)]
    scr_t = sbt("scr", [128, SCR_BYTES], U8)
    ps_t = st.enter_context(nc.psum_tensor("ps", [128, 8, 512], F32))
    PS = Buf(ps_t[:])
    ps = PS.ap

    allocs = []
    cur = [0]

    def reset_scratch():
        cur[0] = 0

    def carve(shape, dt, at=None):
        esz = 4 if dt == F32 else 2
        n = 1
        for s_ in shape[1:]:
            n *= s_
        nbytes = (n * esz + 31) // 32 * 32
        off = cur[0] if at is None else at
        if at is None:
            cur[0] = off + nbytes
        assert off + nbytes <= SCR_BYTES, ("scratch overflow", off + nbytes)
        ap = scr_t[:, off:off + n * esz].bitcast(dt)
        if len(shape) == 3:
            ap = ap.rearrange("p (a b) -> p a b", a=shape[1])
        elif len(shape) == 4:
            ap = ap.rearrange("p (a b c) -> p a b c", a=shape[1], b=shape[2])
        parents = [b_ for (o, e_, b_) in allocs if o < off + nbytes and off < e_]
        b = Buf(ap, parents)
        b.off = off
        allocs.append((off, off + nbytes, b))
        return b

    bank_ctr = [0]
    nbanks = [8]

    def nb():
        b = bank_ctr[0] % nbanks[0]
        bank_ctr[0] += 1
        return b

    def npair():
        if bank_ctr[0] % 2:
            bank_ctr[0] += 1
        b = bank_ctr[0] % 8
        bank_ctr[0] += 2
        return b

    def PB(b):
        return PS.r(b)

    rr = {"cp": 0}

    def copy_op(out_ap, in_ap, reads, writes):
        rr["cp"] ^= 1
        if rr["cp"]:
            pg.add("act", lambda e: e.copy(out_ap, in_ap), reads=reads, writes=writes)
        else:
            pg.add("dve", lambda e: e.tensor_copy(out_ap, in_ap), reads=reads, writes=writes)

    def load(q, out_ap, in_ap, writes):
        return pg.add(q, lambda e: e.dma_start(out=out_ap, in_=in_ap), writes=writes, dma=True)

    def tap(name, ap, shape, dt, regs):
        if name not in tapset:
            return
        d = dram("tap_" + name, shape, dt, out=True)
        op = pg.add("sp", lambda e: e.dma_start(out=d, in_=ap), reads=regs, dma=True)
        pg.final_dmas.append(op)
        tap_out[name] = "tap_" + name

    def xregs0():
        return xT.rs([(0, t) for t in range(5)])

    load("sp", ident.ap, ident_d, [ident.r()])
    load("pool", identb.ap, ident_d, [identb.r()])
    load("sp", cv.ap, cvec_d, [cv.r()])
    load("sp", bada.ap, bada_d, [bada.r()])
    load("sp", nrm.ap, nrm_d, [nrm.r()])
    load("sp", wconv.ap, wconv_d, [wconv.r()])
    load("sp", brs.ap, br_d, [brs.r()])
    pg.add("dve", MEMSET(onesb.ap, 1.0), writes=[onesb.r()])
    load("pool", Ub.ap, U_d, [Ub.r()])
    load("sp", tau.ap, tau_d, [tau.r()])
    load("sp", pconst.ap, pconst_d, [pconst.r()])
    pg.add("act", ACT(scv.ap, cv.ap, AF.Silu), reads=[cv.r()], writes=[scv.r()])

    reset_scratch()
    xs = [carve([128, D], F32) for _ in range(2)]
    wa = [carve([128, 8, 512], BF16) for _ in range(2)]
    mrow = [carve([2, 512], F32) for _ in range(2)]
    for i in range(NT // 128):
        xb = xs[i % 2]
        load("sp", xb.ap, xin[i * 128:(i + 1) * 128, :], [xb.r()])
        t = i // 4
        for half in range(2):
            b = nb()
            fns = [TR(ps[:, b, c4 * 128:(c4 + 1) * 128], xb.ap[:, (half * 4 + c4) * 128:(half * 4 + c4 + 1) * 128], ident.ap) for c4 in range(4)]
            pg.add("pe", seq(fns), reads=[xb.r(), ident.r()], writes=[PB(b)])
            copy_op(xT.ap[:, half * 4:(half + 1) * 4, i * 128:(i + 1) * 128], ps[:, b, :].rearrange("p (a b) -> p a b", a=4),
                    [PB(b)], xT.rs([(c, t) for c in range(half * 4, half * 4 + 4)]))

    zt = carve([128, 8192], BF16)
    HsInit = Buf(Hs_ap)
    RInit = Buf(R_ap)
    HsPrev = [HsInit]
    RPrev = [RInit]
    pg.add("dve", MEMSET(zt.ap, 0.0), writes=[zt.r()])
    for r_ in range(0, NSLOT, 1024):
        n_ = min(1024, NSLOT - r_)
        pg.add("sp", lambda e, o=Hs_ap[r_:r_ + n_, :].rearrange("(p a) n -> p (a n)", a=n_ // 128), i_=zt.ap[:, 0:(n_ // 128) * 1024]: e.dma_start(out=o, in_=i_),
               reads=[zt.r()], writes=[HsInit.r(r_)], dma=True)
    scvb = Buf(sbt("scvb", [128, 16], BF16)[:])
    pg.add("act", ACT(scvb.ap, cv.ap, AF.Silu), reads=[cv.r()], writes=[scvb.r()])

    def compute_mod(li, wab):
        for jb in range(12):
            wb = wab[jb % len(wab)]
            load("pool", wb.ap, w_ada[li, :, jb * 512:(jb + 1) * 512].rearrange("(kc p) n -> p kc n", p=128), [wb.r()])
            b = nb()
            fns = [MM(ps[0:2, b, :], scvb.ap[:, kc * 2:kc * 2 + 2], wb.ap[:, kc, :], start=(kc == 0), stop=(kc == 7)) for kc in range(8)]
            pg.add("pe", seq(fns), reads=[wb.r(), scvb.r()], writes=[PB(b)])
            mr = mrow[jb % 2]
            pg.add("act", lambda e, o=mr.ap[0:2, :], i_=ps[0:2, b, :]: e.copy(o, i_), reads=[PB(b)], writes=[mr.r()])
            b2 = nb()
            fns = [TR(ps[:, b2, j4 * 2:j4 * 2 + 2], mr.ap[0:2, j4 * 128:(j4 + 1) * 128], ident.ap[0:2, 0:2]) for j4 in range(4)]
            pg.add("pe", seq(fns), reads=[mr.r(), ident.r()], writes=[PB(b2)])
            pg.add("dve", TT(mod.ap[:, li, jb * 4:(jb + 1) * 4, :], ps[:, b2, 0:8].rearrange("p (a b) -> p a b", a=4),
                             bada.ap[:, li * 48 + jb * 4:li * 48 + jb * 4 + 4].unsqueeze(2).to_broadcast([128, 4, 2]), ALU.add),
                   reads=[PB(b2), bada.r()], writes=[mod.r(li)])
        for which in range(2):
            base = 8 if which == 0 else 32
            pg.add("dve", STT(gm.ap[:, li, which, :, :], mod.ap[:, li, base:base + 8, :], 1.0,
                              nrm.ap[:, li * 16 + which * 8:li * 16 + which * 8 + 8].unsqueeze(2).to_broadcast([128, 8, 2]),
                              ALU.add, ALU.mult), reads=[mod.r(li), nrm.r()], writes=[gm.r(li)])

    for li_ in range(n_layers):
        compute_mod(li_, wa)
    tap("mod", mod.ap.rearrange("p a b c -> p (a b c)"), [128, 192], F32, [mod.r(0)])
    tap("x0", xT.ap[:, 0, :], [128, NT], F32, xregs0())

    MSH1, MG1, MSH2, MG2 = 0, 16, 24, 40

    NBUF = {}

    def carve_norm():
        NBUF["sq"] = [carve([128, 8, 512], BF16) for _ in range(2)]
        NBUF["tmp"] = [carve([128, 512], F32) for _ in range(4)]
        NBUF["h32"] = [carve([128, 512], F32) for _ in range(4)]
        NBUF["rstd"] = [carve([128, 512], F32) for _ in range(2)]

    def rms_rstd(t, t0, tw):
        b = nb()
        sq = NBUF["sq"][t % 2]
        for hf in range(2):
            pg.add("act", ACT(sq.ap[:, hf * 4:(hf + 1) * 4, 0:tw], xT.ap[:, hf * 4:(hf + 1) * 4, t0:t0 + tw], AF.Square),
                   reads=xT.rs([(c, t) for c in range(hf * 4, hf * 4 + 4)]), writes=[sq.r(hf)])
        pg.add("pe", seq([MM(ps[:, b, 0:tw], onesb.ap, sq.ap[:, c, 0:tw], start=(c == 0), stop=(c == 7)) for c in range(8)]),
               reads=[sq.r(0), sq.r(1), onesb.r()], writes=[PB(b)])
        rs_ = NBUF["rstd"][t % 2]
        pg.add("act", ACT(rs_.ap[:, 0:tw], ps[:, b, 0:tw], AF.Sqrt, scale=1.0 / D, bias=EPS), reads=[PB(b)], writes=[rs_.r()])
        pg.add("dve", RCP(rs_.ap[:, 0:tw], rs_.ap[:, 0:tw]), reads=[rs_.r()], writes=[rs_.r()])
        return rs_

    def norm_phase(li, which, tiles, router=None):
        shb = MSH1 if which == 0 else MSH2
        for (t, (t0, tw)) in tiles:
            s = 0 if t < 4 else 1
            rs_ = rms_rstd(t, t0, tw)
            nch = tw // 128
            rb = [nb() for _ in range(nch)] if router is not None else []
            for c in range(8):
                tm = NBUF["tmp"][c % 4]
                pg.add("dve", STT(tm.ap[:, 0:tw], xT.ap[:, c, t0:t0 + tw], gm.ap[:, li, which, c, s:s + 1], rs_.ap[:, 0:tw], ALU.mult, ALU.mult),
                       reads=[xT.r((c, t)), gm.r(li), rs_.r()], writes=[tm.r()])
                sh_ap = mod.ap[:, li, shb + c, s:s + 1]
                if router is None:
                    pg.add("act", ACT(hT.ap[:, c, t0:t0 + tw], tm.ap[:, 0:tw], AF.Identity, bias=sh_ap, scale=1.0),
                           reads=[tm.r(), mod.r(li)], writes=[hT.r((c, t))])
                else:
                    wr_sb, lg = router
                    hh = NBUF["h32"][c % 4]
                    pg.add("dve", TS(hh.ap[:, 0:tw], tm.ap[:, 0:tw], sh_ap, ALU.add), reads=[tm.r(), mod.r(li)], writes=[hh.r()])
                    pg.add("act", lambda e, o=hT.ap[:, c, t0:t0 + tw], i_=hh.ap[:, 0:tw]: e.copy(o, i_), reads=[hh.r()], writes=[hT.r((c, t))])
                    for i in range(nch):
                        pg.add("pe", MM(ps[:, rb[i], 0:36], hh.ap[:, i * 128:(i + 1) * 128], wr_sb.ap[:, c, :], start=(c == 0), stop=(c == 7)),
                               reads=[hh.r(), wr_sb.r()], writes=[PB(rb[i])])
            if router is not None:
                wr_sb, lg = router
                for i in range(nch):
                    pg.add("dve", TT(lg.ap[:, t * 4 + i, :], ps[:, rb[i], 0:36], brs.ap[:, li * 36:(li + 1) * 36], ALU.add),
                           reads=[PB(rb[i]), brs.r()], writes=[lg.r()])

    def outproj_partial(li, wo, nk, src_fn, src_regs, tiles):
        for (t, (t0, tw)) in tiles:
            s = 0 if t < 4 else 1
            for j in range(8):
                b = nb()
                fns = [MM(ps[:, b, 0:tw], wo.ap[:, k, j * 128:(j + 1) * 128], src_fn(k, t0, tw), start=(k == 0), stop=(k == nk - 1)) for k in range(nk)]
                pg.add("pe", seq(fns), reads=[wo.r()] + src_regs(t), writes=[PB(b)])
                pg.add("dve", STT(xT.ap[:, j, t0:t0 + tw], ps[:, b, 0:tw], mod.ap[:, li, MG1 + j, s:s + 1], xT.ap[:, j, t0:t0 + tw], ALU.mult, ALU.add),
                       reads=[PB(b), mod.r(li), xT.r((j, t))], writes=[xT.r((j, t))])

    def inproj_T(wblk_fn, wreg, tiles, evac):
        for (t, (t0, tw)) in tiles:
            b = nb()
            fns = [MM(ps[:, b, 0:tw], wblk_fn(kc), hT.ap[:, kc, t0:t0 + tw], start=(kc == 0), stop=(kc == 7)) for kc in range(8)]
            pg.add("pe", seq(fns), reads=[wreg] + hT.rs([(kc, t) for kc in range(8)]), writes=[PB(b)])
            evac(t, t0, tw, b)

    for li in range(n_layers):
        last = (li == n_layers - 1)
        tiles_all = list(enumerate(TILES))
        tiles_x = tiles_all[:4]
        tiles_res = tiles_x if last else tiles_all
        nchunks_res = 16 if last else 18

        reset_scratch()
        carve_norm()
        norm_phase(li, 0, tiles_all)
        if li == 0:
            tap("h1", hT.ap[:, 0, :], [128, NT], BF16, hT.rs([(0, t) for t in range(5)]))

        if do_mixer:
            reset_scratch()
            wfb = carve([128, 8, 256], BF16)
            fT = carve([128, 2, NT], BF16)
            wf_sb = carve([128, 2, 256], F32)
            bcs_sb = carve([128, 2, 512], F32)
            WCS = carve([128, 2, 512], BF16)
            PQ = carve([128, 18, 512], BF16)
            tabs = [[carve([128, 16, 256], BF16) for _ in range(2)] for _ in range(2)]
            wo_f = carve([128, 2, D], BF16)
            c2 = carve([128, 2, 256], BF16)
            s2 = carve([128, 2, 256], BF16)
            FwT = carve([128, 2, NT], BF16, at=fT.off)
            load("pool", wfb.ap, w_in[li, :, 0:256].rearrange("(kc p) n -> p kc n", p=128), [wfb.r()])
            load("sp", wf_sb.ap, wf_d[li].rearrange("(kc p) n -> p kc n", p=128), [wf_sb.r()])
            load("sp", bcs_sb.ap, BCS_d.rearrange("(kc p) n -> p kc n", p=128), [bcs_sb.r()])
            load("pool", wo_f.ap, w_out[li, 0:256, :].rearrange("(kc p) n -> p kc n", p=128), [wo_f.r()])
            for mi in range(2):
                b = nb()
                fns = []
                for cs_ in range(2):
                    for kc in range(2):
                        fns.append(MM(ps[:, b, cs_ * 256:(cs_ + 1) * 256], bcs_sb.ap[:, kc, cs_ * 256 + mi * 128:cs_ * 256 + (mi + 1) * 128],
                                      wf_sb.ap[:, kc, :], start=(kc == 0), stop=(kc == 1)))
                pg.add("pe", seq(fns), reads=[bcs_sb.r(), wf_sb.r()], writes=[PB(b)])
                copy_op(WCS.ap[:, mi, :], ps[:, b, :], [PB(b)], [WCS.r()])
            for fc in range(2):
                def evac_f(t, t0, tw, b, fc=fc):
                    copy_op(fT.ap[:, fc, t0:t0 + tw], ps[:, b, 0:tw], [PB(b)], [fT.r((fc, t))])
                inproj_T((lambda fc: lambda kc: wfb.ap[:, kc, fc * 128:(fc + 1) * 128])(fc), wfb.r(), tiles_res, evac_f)
            for i in range(nchunks_res):
                b = nb()
                fns = [MM(ps[:, b, :], fT.ap[:, fc, i * 128:(i + 1) * 128], WCS.ap[:, fc, :], start=(fc == 0), stop=(fc == 1)) for fc in range(2)]
                pg.add("pe", seq(fns), reads=[WCS.r()] + fT.rs([(0, i // 4), (1, i // 4)]), writes=[PB(b)])
                copy_op(PQ.ap[:, i, :], ps[:, b, :], [PB(b)], [PQ.r(i)])
            for kt in range(8):
                tb = tabs[kt % 2]
                load("sp", tb[0].ap, CN_d[:, kt * 256:(kt + 1) * 256].rearrange("(nc p) k -> p nc k", p=128), [tb[0].r()])
                load("sp", tb[1].ap, SN_d[:, kt * 256:(kt + 1) * 256].rearrange("(nc p) k -> p nc k", p=128), [tb[1].r()])
                for fc in range(2):
                    b = nb()
                    fns = []
                    for n_ in range(16):
                        for cs_ in range(2):
                            fns.append(MM(ps[:, b, 0:256], PQ.ap[:, n_, cs_ * 256 + fc * 128:cs_ * 256 + (fc + 1) * 128], tb[cs_].ap[:, n_, :],
                                          start=(n_ == 0 and cs_ == 0), stop=(n_ == 15 and cs_ == 1)))
                    pg.add("pe", seq(fns), reads=[tb[0].r(), tb[1].r()] + PQ.rs(range(16)), writes=[PB(b)])
                    copy_op(FwT.ap[:, fc, kt * 256:(kt + 1) * 256], ps[:, b, 0:256], [PB(b)], [FwT.r(kt // 2)])
            if not last:
                load("sp", c2.ap, C2_d.rearrange("(nc p) k -> p nc k", p=128), [c2.r()])
                load("sp", s2.ap, S2_d.rearrange("(nc p) k -> p nc k", p=128), [s2.r()])
                for fc in range(2):
                    b = nb()
                    fns = []
                    for n_ in range(2):
                        for cs_, tb_ in ((0, c2), (1, s2)):
                            fns.append(MM(ps[:, b, 0:256], PQ.ap[:, 16 + n_, cs_ * 256 + fc * 128:cs_ * 256 + (fc + 1) * 128], tb_.ap[:, n_, :],
                                          start=(n_ == 0 and cs_ == 0), stop=(n_ == 1 and cs_ == 1)))
                    pg.add("pe", seq(fns), reads=[c2.r(), s2.r()] + PQ.rs([16, 17]), writes=[PB(b)])
                    copy_op(FwT.ap[:, fc, 2048:2304], ps[:, b, 0:256], [PB(b)], [FwT.r(4)])
            if li == 0:
                tap("FwT", FwT.ap[:, 0, :], [128, NT], BF16, FwT.rs(range(5)))
            outproj_partial(li, wo_f, 2, lambda k, t0, tw: FwT.ap[:, k, t0:t0 + tw], lambda t: [FwT.r(t)], tiles_res)
            if li == 0:
                tap("xF", xT.ap[:, 0, :], [128, NT], F32, xregs0())

            reset_scratch()
            wg = carve([128, 8, 768], BF16)
            zT = carve([128, 2, NT], BF16)
            gbT = carve([128, 2, NT], BF16)
            convT = carve([128, 2, NT], BF16)
            cacc = carve([128, NT], F32)
            gtmp = [carve([128, 512], F32) for _ in range(2)]
            wo_c = carve([128, 2, D], BF16)
            load("pool", wg.ap, w_in[li, :, 1792:2560].rearrange("(kc p) n -> p kc n", p=128), [wg.r()])
            load("pool", wo_c.ap, w_out[li, 768:1024, :].rearrange("(kc p) n -> p kc n", p=128), [wo_c.r()])
            for cc in range(2):
                def evac_gb(t, t0, tw, b, cc=cc):
                    copy_op(gbT.ap[:, cc, t0:t0 + tw], ps[:, b, 0:tw], [PB(b)], [gbT.r((cc, t))])
                inproj_T((lambda cc: lambda kc: wg.ap[:, kc, cc * 128:(cc + 1) * 128])(cc), wg.r(), tiles_res, evac_gb)

                def evac_gc(t, t0, tw, b, cc=cc):
                    g = gtmp[t % 2]
                    pg.add("act", lambda e, o=g.ap[:, 0:tw], i_=ps[:, b, 0:tw]: e.copy(o, i_), reads=[PB(b)], writes=[g.r()])
                    b2 = nb()
                    fns = [MM(ps[:, b2, 0:tw], wg.ap[:, kc, 512 + cc * 128:512 + (cc + 1) * 128], hT.ap[:, kc, t0:t0 + tw],
                              start=(kc == 0), stop=(kc == 7)) for kc in range(8)]
                    pg.add("pe", seq(fns), reads=[wg.r()] + hT.rs([(kc, t) for kc in range(8)]), writes=[PB(b2)])
                    pg.add("dve", TT(zT.ap[:, cc, t0:t0 + tw], g.ap[:, 0:tw], ps[:, b2, 0:tw], ALU.mult), reads=[g.r(), PB(b2)], writes=[zT.r((cc, t))])
                inproj_T((lambda cc: lambda kc: wg.ap[:, kc, 256 + cc * 128:256 + (cc + 1) * 128])(cc), wg.r(), tiles_res, evac_gc)
                segs = [(0, NX)] if last else [(0, NX), (NX, NT)]
                zregs = zT.rs([(cc, t) for (t, _) in tiles_res])
                wbase = li * 6 + cc * 3
                w0 = wconv.ap[:, wbase + 0:wbase + 1]
                w1 = wconv.ap[:, wbase + 1:wbase + 2]
                w2 = wconv.ap[:, wbase + 2:wbase + 3]
                for (a0, a1) in segs:
                    pg.add("dve", TS(cacc.ap[:, a0:a1], zT.ap[:, cc, a0:a1], w1, ALU.mult), reads=zregs + [wconv.r()], writes=[cacc.r()])
                    pg.add("dve", STT(cacc.ap[:, a0 + 1:a1], zT.ap[:, cc, a0:a1 - 1], w0, cacc.ap[:, a0 + 1:a1], ALU.mult, ALU.add),
                           reads=zregs + [wconv.r(), cacc.r()], writes=[cacc.r()])
                    pg.add("dve", STT(cacc.ap[:, a0:a1 - 1], zT.ap[:, cc, a0 + 1:a1], w2, cacc.ap[:, a0:a1 - 1], ALU.mult, ALU.add),
                           reads=zregs + [wconv.r(), cacc.r()], writes=[cacc.r()])
                    pg.add("dve", TT(convT.ap[:, cc, a0:a1], cacc.ap[:, a0:a1], gbT.ap[:, cc, a0:a1], ALU.mult),
                           reads=[cacc.r()] + gbT.rs([(cc, t) for (t, _) in tiles_res]), writes=[convT.r(cc)])
            if li == 0:
                tap("convT", convT.ap[:, 0, :], [128, NT], BF16, convT.rs([0, 1]))
            outproj_partial(li, wo_c, 2, lambda k, t0, tw: convT.ap[:, k, t0:t0 + tw], lambda t: convT.rs([0, 1]), tiles_res)
            if li == 0:
                tap("xC", xT.ap[:, 0, :], [128, NT], F32, xregs0())

            reset_scratch()
            qT = carve([128, NT], BF16)
            kTm = [carve([128, NT], BF16) for _ in range(2)]
            Va = carve([128, 18, 2, 65], BF16)
            attn = carve([128, 18, 512], BF16)
            biasb = carve([128, 2, 5, 640], BF16)
            tmpS = [carve([128, 640], F32) for _ in range(2)]
            PT = [carve([128, 7, 128], BF16) for _ in range(2)]
            attnT = carve([128, 4, 512], BF16)
            wo_a = carve([128, 4, D], BF16)
            wqkv = [carve([128, 3, 8, 128], BF16) for _ in range(2)]
            rec = [carve([128, 2], F32) for _ in range(2)]
            load("pool", wo_a.ap, w_out[li, 256:768, :].rearrange("(kc p) n -> p kc n", p=128), [wo_a.r()])
            pg.add("dve", MEMSET(Va.ap[:, :, :, 64:65], 1.0), writes=[Va.r("ones")])
            pg.add("dve", MEMSET(kTm[0].ap[64:128, :], 0.0), writes=[kTm[0].r("z")])
            pg.add("dve", MEMSET(kTm[1].ap[0:64, :], 0.0), writes=[kTm[1].r("z")])
            nqb = 16 if last else 18
            for hp in range(4):
                wq = wqkv[hp % 2]
                for m, col0 in enumerate((256, 768, 1280)):
                    load("pool", wq.ap[:, m, :, :], w_in[li, :, col0 + hp * 128:col0 + (hp + 1) * 128].rearrange("(kc p) n -> p kc n", p=128), [wq.r(m)])

                def evac_q(t, t0, tw, b):
                    pg.add("act", ACT(qT.ap[:, t0:t0 + tw], ps[:, b, 0:tw], AF.Identity, scale=0.125), reads=[PB(b)], writes=[qT.r(t)])

                def evac_k(t, t0, tw, b):
                    pg.add("dve", lambda e, o=kTm[0].ap[0:64, t0:t0 + tw], i_=ps[0:64, b, 0:tw]: e.tensor_copy(o, i_), reads=[PB(b)], writes=[kTm[0].r(t)])
                    pg.add("act", lambda e, o=kTm[1].ap[64:128, t0:t0 + tw], i_=ps[64:128, b, 0:tw]: e.copy(o, i_), reads=[PB(b)], writes=[kTm[1].r(t)])
                inproj_T((lambda wq: lambda kc: wq.ap[:, 0, kc, :])(wq), wq.r(0), tiles_res, evac_q)
                inproj_T((lambda wq: lambda kc: wq.ap[:, 1, kc, :])(wq), wq.r(1), tiles_all, evac_k)
                for i4 in range(0, 18, 4):
                    b = nb()
                    n4 = min(4, 18 - i4)
                    fns = []
                    for ii in range(n4):
                        i = i4 + ii
                        for kc in range(8):
                            fns.append(MM(ps[:, b, ii * 128:(ii + 1) * 128], hT.ap[:, kc, i * 128:(i + 1) * 128], wq.ap[:, 2, kc, :],
                                          start=(kc == 0), stop=(kc == 7)))
                    pg.add("pe", seq(fns), reads=[wq.r(2)] + hT.rs([(kc, i4 // 4) for kc in range(8)]), writes=[PB(b)])
                    copy_op(Va.ap[:, i4:i4 + n4, :, 0:64], ps[:, b, 0:n4 * 128].rearrange("p (a h d) -> p a h d", a=n4, h=2),
                            [PB(b)], [Va.r(i4 // 4)])
                vregs_all = [Va.r("ones")] + Va.rs(range(5))
                for hh in range(2):
                    load("pool", biasb.ap[:, hh, :, :].rearrange("p a b -> p (a b)"), biasT_d[li, hp * 2 + hh], [biasb.r(hh)])
                for j in range(nqb):
                    isx = j < 16
                    kchunks = ([cs_of(j) + i for i in range(5)] + [16, 17]) if isx else [16, 17]
                    nk = len(kchunks)
                    ptr = []
                    for hh in range(2):
                        bp = npair()
                        fns = []
                        for ki, kc_ in enumerate(kchunks):
                            o_ = ps[:, bp + ki // 4, (ki % 4) * 128:(ki % 4 + 1) * 128]
                            if isx and ki < 5:
                                fns.append(MM(o_, kTm[hh].ap[:, kc_ * 128:(kc_ + 1) * 128], qT.ap[:, j * 128:(j + 1) * 128], start=True, stop=False))
                                fns.append(MM(o_, identb.ap, biasb.ap[:, hh, cls_of(j), ki * 128:(ki + 1) * 128], start=False, stop=True))
                            else:
                                fns.append(MM(o_, kTm[hh].ap[:, kc_ * 128:(kc_ + 1) * 128], qT.ap[:, j * 128:(j + 1) * 128]))
                        pg.add("pe", seq(fns), reads=[qT.r(j // 4), kTm[hh].r("z"), identb.r(), biasb.r(hh)] + kTm[hh].rs(sorted(set(k_ // 4 for k_ in kchunks))),
                               writes=[PB(bp), PB(bp + 1)])
                        pt = PT[hh]
                        sview = ps[:, bp:bp + 2, :].rearrange("p a b -> p (a b)")
                        if isx:
                            pg.add("act", ACT(pt.ap.rearrange("p a b -> p (a b)"), sview[:, 0:896], AF.Exp),
                                   reads=[PB(bp), PB(bp + 1)], writes=[pt.r("l"), pt.r("c")])
                            ptr.append([pt.r("l"), pt.r("c")])
                        else:
                            pg.add("act", ACT(pt.ap[:, 0:2, :].rearrange("p a b -> p (a b)"), sview[:, 0:256], AF.Exp),
                                   reads=[PB(bp), PB(bp + 1)], writes=[pt.r("l")])
                            ptr.append([pt.r("l")])
                    bo = nb()
                    for hh in range(2):
                        fns = [MM(ps[:, bo, hh * 65:(hh + 1) * 65], PT[hh].ap[:, ki, :], Va.ap[:, kc_, hh, :], start=(ki == 0), stop=(ki == nk - 1))
                               for ki, kc_ in enumerate(kchunks)]
                        pg.add("pe", seq(fns), reads=ptr[hh] + vregs_all, writes=[PB(bo)])
                    rc = rec[j % 2]
                    pv3 = ps[:, bo, 0:130].rearrange("p (h d) -> p h d", h=2)
                    pg.add("dve", RCP(rc.ap[:, 0:2].unsqueeze(2), pv3[:, :, 64:65]), reads=[PB(bo)], writes=[rc.r()])
                    pg.add("dve", TT(attn.ap[:, j, hp * 128:(hp + 1) * 128].rearrange("p (h d) -> p h d", h=2), pv3[:, :, 0:64],
                                     rc.ap[:, 0:2].unsqueeze(2).to_broadcast([128, 2, 64]), ALU.mult),
                           reads=[PB(bo), rc.r()], writes=[attn.r((j // 4, hp * 2)), attn.r((j // 4, hp * 2 + 1))])
            if li == 0:
                tap("attn", attn.ap.rearrange("p a b -> p (a b)"), [128, 18 * 512], BF16, attn.allregs())
            for (t, (t0, tw)) in tiles_res:
                nch = tw // 128
                for a in range(4):
                    b = nb()
                    pbv = ps[:, b, :].bitcast(BF16)
                    fns = [TR(pbv[:, ii * 128:(ii + 1) * 128], attn.ap[:, t * 4 + ii, a * 128:(a + 1) * 128], identb.ap) for ii in range(nch)]
                    pg.add("pe", seq(fns), reads=[identb.r()] + attn.rs([(t, 2 * a), (t, 2 * a + 1)]), writes=[PB(b)])
                    copy_op(attnT.ap[:, a, 0:tw], pbv[:, 0:tw], [PB(b)], [attnT.r(a)])
                outproj_partial(li, wo_a, 4, lambda k, t0, tw: attnT.ap[:, k, 0:tw], lambda t: attnT.rs(range(4)), [(t, (t0, tw))])
            if li == 0:
                tap("xM", xT.ap[:, 0, :], [128, NT], F32, xregs0())

        reset_scratch()
        n_ = nchunks_res
        NTL = NTILES[0] if not last else NTILES[1]
        lg = carve([128, 18, 36], F32)
        wr_sb = carve([128, 8, 36], F32)
        r_gmax = carve([128, 18], F32)
        r_goh = carve([128, 18, 4], F32)
        r_gex = carve([128, 18, 4], F32)
        r_gw = carve([128, 18], F32)
        r_em = carve([128, 18, 4, 8], F32)
        r_es = carve([128, 18, 8], F32)
        r_m1 = carve([128, 18], F32)
        r_o1 = carve([128, 18, 8], F32)
        r_e2 = carve([128, 18, 8], F32)
        r_m2 = carve([128, 18], F32)
        r_o2 = carve([128, 18, 8], F32)
        r_d = carve([128, 18], F32)
        r_w1 = carve([128, 18], F32)
        A1 = carve([128, 18, 4, 8], F32)
        A2 = carve([128, 18, 4, 8], F32)
        Mb = carve([128, 18, 32], BF16)
        rank = carve([128, 18, 32], F32)
        tmpk = carve([128, 18, 32], F32)
        cnt = carve([128, 32], F32)
        ntl = carve([128, 32], F32)
        pa = carve([128, 32], F32)
        pb_ = carve([128, 32], F32)
        segst = carve([128, 32], F32)
        posf = [carve([128, 18], F32) for _ in range(2)]
        cmpb = carve([128, NTMAX, 32], F32)
        etl = carve([128, NTMAX], F32)
        htm = [carve([128, D], BF16) for _ in range(2)]

        carve_norm()
        load("sp", wr_sb.ap, wr_d[li].rearrange("(kc p) n -> p kc n", p=128), [wr_sb.r()])
        norm_phase(li, 1, tiles_res, router=(wr_sb, lg))

        def V(b_):
            return b_.ap[:, 0:n_]
        G = lg.ap[:, 0:n_, 0:4]
        E4 = lg.ap[:, 0:n_, 4:36].rearrange("p n (g e) -> p n g e", g=4)

        def dv(fn, reads, writes):
            pg.add("dve", fn, reads=[x_.r() for x_ in reads], writes=[x_.r() for x_ in writes])

        def bc3(b_, k):
            return V(b_).unsqueeze(2).to_broadcast([128, n_, k])

        pw = [pw1[li], pw2[li]]
        dv(RED(V(r_gmax), G, ALU.max), [lg], [r_gmax])
        dv(TT(V(r_goh), G, bc3(r_gmax, 4), ALU.is_equal), [lg, r_gmax], [r_goh])
        dv(TT(V(r_gex), G, bc3(r_gmax, 4), ALU.subtract), [lg, r_gmax], [r_gex])
        pg.add("act", ACT(V(r_gex), V(r_gex), AF.Exp), reads=[r_gex.r()], writes=[r_gex.r()])
        dv(RED(V(r_gw), V(r_gex), ALU.add), [r_gex], [r_gw])
        dv(RCP(V(r_gw), V(r_gw)), [r_gw], [r_gw])
        dv(TT(V(r_em), E4, V(r_goh).unsqueeze(3).to_broadcast([128, n_, 4, 8]), ALU.mult), [lg, r_goh], [r_em])
        dv(RED(V(r_es), V(r_em).rearrange("p n g e -> p n e g"), ALU.add), [r_em], [r_es])
        dv(RED(V(r_m1), V(r_es), ALU.max), [r_es], [r_m1])
        dv(TT(V(r_o1), V(r_es), bc3(r_m1, 8), ALU.is_equal), [r_es, r_m1], [r_o1])
        dv(STT(V(r_e2), V(r_o1), -1e30, V(r_es), ALU.mult, ALU.add), [r_o1, r_es], [r_e2])
        dv(RED(V(r_m2), V(r_e2), ALU.max), [r_e2], [r_m2])
        dv(TT(V(r_o2), V(r_e2), bc3(r_m2, 8), ALU.is_equal), [r_e2, r_m2], [r_o2])
        dv(TT(V(r_d), V(r_m2), V(r_m1), ALU.subtract), [r_m1, r_m2], [r_d])
        pg.add("act", ACT(V(r_d), V(r_d), AF.Exp), reads=[r_d.r()], writes=[r_d.r()])
        dv(TS(V(r_w1), V(r_d), 1.0, ALU.add), [r_d], [r_w1])
        dv(RCP(V(r_w1), V(r_w1)), [r_w1], [r_w1])
        dv(TT(V(pw[1]), V(r_d), V(r_w1), ALU.mult), [r_d, r_w1], [pw[1]])
        dv(TT(V(pw[0]), V(r_w1), V(r_gw), ALU.mult), [r_w1, r_gw], [pw[0]])
        dv(TT(V(pw[1]), V(pw[1]), V(r_gw), ALU.mult), [pw[1], r_gw], [pw[1]])
        gohb = V(r_goh).unsqueeze(3).to_broadcast([128, n_, 4, 8])
        dv(TT(V(A1), gohb, V(r_o1).unsqueeze(2).to_broadcast([128, n_, 4, 8]), ALU.mult), [r_goh, r_o1], [A1])
        dv(TT(V(A2), gohb, V(r_o2).unsqueeze(2).to_broadcast([128, n_, 4, 8]), ALU.mult), [r_goh, r_o2], [A2])
        A1f = V(A1).rearrange("p n g e -> p n (g e)")
        A2f = V(A2).rearrange("p n g e -> p n (g e)")
        dv(TT(V(Mb), A1f, A2f, ALU.add), [A1, A2], [Mb])
        if li == 0:
            tap("lg", lg.ap.rearrange("p a b -> p (a b)"), [128, 18 * 36], F32, [lg.r()])
        brk = [nb(), nb()]
        for bi in range(2):
            lo, hi = bi * 16, min(n_, bi * 16 + 16)
            if lo >= hi:
                continue
            fns = []
            for i in range(lo, hi):
                col = (i - lo) * 32
                for i2 in range(i):
                    fns.append(MM(ps[:, brk[bi], col:col + 32], onesb.ap, Mb.ap[:, i2, :], start=(i2 == 0), stop=False))
                fns.append(MM(ps[:, brk[bi], col:col + 32], Ub.ap, Mb.ap[:, i, :], start=(i == 0), stop=True))
            pg.add("pe", seq(fns), reads=[onesb.r(), Ub.r(), Mb.r()], writes=[PB(brk[bi])])
            copy_op(rank.ap[:, lo:hi, :], ps[:, brk[bi], 0:(hi - lo) * 32].rearrange("p (a b) -> p a b", b=32), [PB(brk[bi])], [rank.r(bi)])
        bcn = nb()
        pg.add("pe", seq([MM(ps[:, bcn, 0:32], onesb.ap, Mb.ap[:, i, :], start=(i == 0), stop=(i == n_ - 1)) for i in range(n_)]),
               reads=[onesb.r(), Mb.r()], writes=[PB(bcn)])
        pg.add("dve", lambda e, o=cnt.ap, i_=ps[:, bcn, 0:32]: e.tensor_copy(o, i_), reads=[PB(bcn)], writes=[cnt.r()])
        dv(TS(ntl.ap, cnt.ap, 0.0, ALU.is_gt), [cnt], [ntl])
        for m in range(1, (NT + TW - 1) // TW):
            dv(STT(ntl.ap, cnt.ap, float(TW * m), ntl.ap, ALU.is_gt, ALU.add), [cnt, ntl], [ntl])
        src, dst = ntl, pa
        for sh in (1, 2, 4, 8, 16):
            dv(lambda e, o=dst.ap[:, 0:sh], i_=src.ap[:, 0:sh]: e.tensor_copy(o, i_), [src], [dst])
            dv(TT(dst.ap[:, sh:32], src.ap[:, sh:32], src.ap[:, 0:32 - sh], ALU.add), [src, dst], [dst])
            src, dst = dst, (pb_ if dst is pa else pa)
        incl = src
        dv(TT(segst.ap, incl.ap, ntl.ap, ALU.subtract), [incl, ntl], [segst])
        dv(TS(segst.ap, segst.ap, float(TW), ALU.mult), [segst], [segst])
        pg.add("dve", TT(V(rank), V(rank), segst.ap.unsqueeze(1).to_broadcast([128, n_, 32]), ALU.add),
               reads=[rank.r(0), rank.r(1), segst.r()], writes=[rank.r(0), rank.r(1)])
        for k, Af in ((0, A1f), (1, A2f)):
            pg.add("dve", TT(V(tmpk), V(rank), Af, ALU.mult), reads=rank.allregs() + [A1.r(), A2.r()], writes=[tmpk.r()])
            dv(RED(V(posf[k]), V(tmpk), ALU.add), [tmpk], [posf[k]])
            dv(lambda e, o=V(posi[li][k]), i_=V(posf[k]): e.tensor_copy(o, i_), [posf[k]], [posi[li][k]])
        dv(TT(cmpb.ap[:, 0:NTL, :], incl.ap.unsqueeze(1).to_broadcast([128, NTL, 32]), tau.ap[:, 0:NTL].unsqueeze(2).to_broadcast([128, NTL, 32]), ALU.is_le),
           [incl, tau], [cmpb])
        dv(RED(etl.ap[:, 0:NTL], cmpb.ap[:, 0:NTL, :], ALU.add), [cmpb], [etl])
        dv(TS(etl.ap[:, 0:NTL], etl.ap[:, 0:NTL], 31.0, ALU.min), [etl], [etl])
        dv(lambda e, o=etl.ap[:, 0:NTL], c_=pconst.ap[:, li:li + 1]: e.tensor_scalar(out=o, in0=o, scalar1=128.0, scalar2=c_, op0=ALU.mult, op1=ALU.add),
           [etl, pconst], [etl])
        dv(lambda e, o=widx[li].ap[:, 0:NTL], i_=etl.ap[:, 0:NTL]: e.tensor_copy(o, i_), [etl], [widx[li]])
        if li == 0:
            tap("pos1", posf[0].ap, [128, 18], F32, [posf[0].r()])
            tap("pos2", posf[1].ap, [128, 18], F32, [posf[1].r()])
            tap("etl", etl.ap, [128, NTMAX], F32, [etl.r()])
        HsL = Buf(Hs_ap, parents=[HsPrev[0]])
        for i in range(n_):
            b = nb()
            pbv = ps[:, b, :].bitcast(BF16)
            fns = [TR(pbv[:, kc * 128:(kc + 1) * 128], hT.ap[:, kc, i * 128:(i + 1) * 128], identb.ap) for kc in range(8)]
            pg.add("pe", seq(fns), reads=[identb.r()] + hT.rs([(kc, i // 4) for kc in range(8)]), writes=[PB(b)])
            hb_ = htm[i % 2]
            copy_op(hb_.ap, pbv, [PB(b)], [hb_.r()])
            for k in range(2):
                pg.add("pool", lambda e, i_=hb_.ap, off=posi[li][k].ap[:, i:i + 1]: e.indirect_dma_start(
                    out=Hs_ap, out_offset=bass.IndirectOffsetOnAxis(ap=off, axis=0), in_=i_, in_offset=None),
                    reads=[hb_.r(), posi[li][k].r()], writes=[HsL.r((i, k))], dma=True)
        HsPrev[0] = HsL
        hs_regs = HsL.allregs()

        reset_scratch()
        wgu = [carve([128, 8192], BF16) for _ in range(2)]
        wdn = [carve([128, 4096], BF16) for _ in range(2)]
        hid = [carve([128, 4, TW], BF16) for _ in range(2)]
        sg = [carve([128, TW], BF16) for _ in range(3)]
        rst = [carve([128, D], F32) for _ in range(2)]
        hg = [Buf(hT_t[:, q * 4096:q * 4096 + 8 * TW].rearrange("p (a b) -> p a b", a=8), parents=[hT]) for q in range(2)]
        hst = [Buf(hT_t[:, 8192 + q * 4096:8192 + q * 4096 + NSC * 1024].rearrange("p (a b) -> p a b", a=NSC), parents=[hT]) for q in range(2)]
        RL = Buf(R_ap, parents=[RPrev[0]])

        def load_dn(tt):
            ws = wdn[tt % 2]
            pg.add("pool", lambda e, o=ws.ap, off=widx[li].ap[:, tt:tt + 1]: e.indirect_dma_start(
                out=o, out_offset=None, in_=W_dn, in_offset=bass.IndirectOffsetOnAxis(ap=off, axis=0)),
                reads=[widx[li].r()], writes=[ws.r()], dma=True)

        def tile_loads(tt):
            ws = wgu[tt % 2]
            pg.add("pool", lambda e, o=ws.ap, off=widx[li].ap[:, tt:tt + 1]: e.indirect_dma_start(
                out=o, out_offset=None, in_=W_gu, in_offset=bass.IndirectOffsetOnAxis(ap=off, axis=0)),
                reads=[widx[li].r()], writes=[ws.r()], dma=True)
            hs_ = hst[tt % 2]
            pg.add("sp", lambda e, o=hs_.ap, i_=Hs_ap[tt * TW:(tt + 1) * TW, :].rearrange("(c p) n -> p c n", p=128): e.dma_start(out=o, in_=i_),
                   reads=hs_regs, writes=[hs_.r()], dma=True)

        def tile_front(tt):
            ws = wgu[tt % 2]
            hs_ = hst[tt % 2]
            hg_ = hg[tt % 2]
            hb = hid[tt % 2]
            for kc in range(8):
                b = nb()
                pbv = ps[:, b, :].bitcast(BF16)
                fns = [TR(pbv[:, c * 128:(c + 1) * 128], hs_.ap[:, c, kc * 128:(kc + 1) * 128], identb.ap) for c in range(NSC)]
                pg.add("pe", seq(fns), reads=[identb.r(), hs_.r()], writes=[PB(b)])
                copy_op(hg_.ap[:, kc, :], pbv[:, 0:TW], [PB(b)], [hg_.r(kc)])
            hregs = hg_.rs(range(8))
            wg_ = ws.ap[:, 0:4096].rearrange("p (a b) -> p a b", a=8)
            wu_ = ws.ap[:, 4096:8192].rearrange("p (a b) -> p a b", a=8)
            for dc in range(4):
                bg = nb()
                bu = nb()
                fg = [MM(ps[:, bg, 0:TW], wg_[:, kc, dc * 128:(dc + 1) * 128], hg_.ap[:, kc, :], start=(kc == 0), stop=(kc == 7)) for kc in range(8)]
                fu = [MM(ps[:, bu, 0:TW], wu_[:, kc, dc * 128:(dc + 1) * 128], hg_.ap[:, kc, :], start=(kc == 0), stop=(kc == 7)) for kc in range(8)]
                pg.add("pe", seq(fg), reads=[ws.r()] + hregs, writes=[PB(bg)])
                pg.add("pe", seq(fu), reads=[ws.r()] + hregs, writes=[PB(bu)])
                sgb = sg[dc % 3]
                pg.add("act", ACT(sgb.ap, ps[:, bg, 0:TW], AF.Silu), reads=[PB(bg)], writes=[sgb.r()])
                pg.add("dve", TT(hb.ap[:, dc, :], sgb.ap, ps[:, bu, 0:TW], ALU.mult), reads=[sgb.r(), PB(bu)], writes=[hb.r(dc)])

        def tile_back(tt):
            ws = wdn[tt % 2]
            hb = hid[tt % 2]
            wd_ = ws.ap.rearrange("p (a b) -> p a b", a=4)
            for sc_ in range(NSC):
                ro = rst[(tt * NSC + sc_) % 2]
                for half in range(2):
                    b = nb()
                    fns = [MM(ps[:, b, :], hb.ap[:, dc, sc_ * 128:(sc_ + 1) * 128], wd_[:, dc, half * 512:(half + 1) * 512], start=(dc == 0), stop=(dc == 3))
                           for dc in range(4)]
                    pg.add("pe", seq(fns), reads=[ws.r()] + hb.rs(range(4)), writes=[PB(b)])
                    copy_op(ro.ap[:, half * 512:(half + 1) * 512], ps[:, b, :], [PB(b)], [ro.r(half)])
                r0 = tt * TW + sc_ * 128
                pg.add("sp", lambda e, o=R_ap[r0:r0 + 128, :], i_=ro.ap: e.dma_start(out=o, in_=i_), reads=[ro.r(0), ro.r(1)], writes=[RL.r((tt, sc_))], dma=True)

        tile_loads(0)
        load_dn(0)
        for tt in range(NTL):
            if tt + 1 < NTL:
                tile_loads(tt + 1)
            tile_front(tt)
            if tt > 0:
                tile_back(tt - 1)
            if tt + 1 < NTL:
                load_dn(tt + 1)
        tile_back(NTL - 1)
        RPrev[0] = RL
        r_regs = RL.allregs()

        reset_scratch()
        Gb = [[carve([128, D], F32) for _ in range(2)] for _ in range(2)]
        yb = [carve([128, D], F32) for _ in range(2)]
        for i in range(n_):
            t = i // 4
            s = 0 if t < 4 else 1
            for k in range(2):
                g_ = Gb[k][i % 2]
                pg.add("pool", lambda e, o=g_.ap, off=posi[li][k].ap[:, i:i + 1]: e.indirect_dma_start(
                    out=o, out_offset=None, in_=R_ap, in_offset=bass.IndirectOffsetOnAxis(ap=off, axis=0)),
                    reads=r_regs + [posi[li][k].r()], writes=[g_.r()], dma=True)
            y_ = yb[i % 2]
            pg.add("dve", TS(y_.ap, Gb[0][i % 2].ap, pw[0].ap[:, i:i + 1], ALU.mult), reads=[Gb[0][i % 2].r(), pw[0].r()], writes=[y_.r()])
            pg.add("dve", STT(y_.ap, Gb[1][i % 2].ap, pw[1].ap[:, i:i + 1], y_.ap, ALU.mult, ALU.add), reads=[Gb[1][i % 2].r(), pw[1].r(), y_.r()], writes=[y_.r()])
            for half in range(2):
                b = nb()
                fns = [TR(ps[:, b, c4 * 128:(c4 + 1) * 128], y_.ap[:, (half * 4 + c4) * 128:(half * 4 + c4 + 1) * 128], ident.ap) for c4 in range(4)]
                pg.add("pe", seq(fns), reads=[ident.r(), y_.r()], writes=[PB(b)])
                for c4 in range(4):
                    j = half * 4 + c4
                    pg.add("dve", STT(xT.ap[:, j, i * 128:(i + 1) * 128], ps[:, b, c4 * 128:(c4 + 1) * 128], mod.ap[:, li, MG2 + j, s:s + 1],
                                      xT.ap[:, j, i * 128:(i + 1) * 128], ALU.mult, ALU.add),
                           reads=[PB(b), mod.r(li), xT.r((j, t))], writes=[xT.r((j, t))])
        for g in hT.allregs():
            _inherit(g, [r_ for b_ in hg + hst for r_ in b_.allregs()])
        if li == 0:
            tap("x1", xT.ap[:, 0, :], [128, NT], F32, xregs0())

    reset_scratch()
    yT = carve([128, 8, 512], F32)
    ob = [carve([128, D], F32) for _ in range(2)]
    carve_norm()
    nfb = 32
    for (t, (t0, tw)) in list(enumerate(TILES))[:4]:
        rs_ = rms_rstd(t, t0, tw)
        for c in range(8):
            pg.add("dve", STT(yT.ap[:, c, 0:tw], xT.ap[:, c, t0:t0 + tw], nrm.ap[:, nfb + c:nfb + c + 1], rs_.ap[:, 0:tw], ALU.mult, ALU.mult),
                   reads=[xT.r((c, t)), nrm.r(), rs_.r()], writes=[yT.r(c)])
        for ii in range(4):
            i = t * 4 + ii
            o_ = ob[i % 2]
            for half in range(2):
                b = nb()
                fns = [TR(ps[:, b, c4 * 128:(c4 + 1) * 128], yT.ap[:, half * 4 + c4, ii * 128:(ii + 1) * 128], ident.ap) for c4 in range(4)]
                pg.add("pe", seq(fns), reads=[ident.r()] + yT.rs(range(half * 4, half * 4 + 4)), writes=[PB(b)])
                copy_op(o_.ap[:, half * 512:(half + 1) * 512], ps[:, b, :], [PB(b)], [o_.r(half)])
            op = pg.add("sp", lambda e, o=out_d[i * 128:(i + 1) * 128, :], i_=o_.ap: e.dma_start(out=o, in_=i_), reads=[o_.r(0), o_.r(1)], dma=True)
            pg.final_dmas.append(op)

    pg.emit(st)
    st.close()
    return nc, tap_out


_CONST = {}


def _constants():
    if _CONST:
        return _CONST
    bf = ml_dtypes.bfloat16
    n = np.arange(NX)
    ang = 2.0 * np.pi * ((n[:, None] * n[None, :]) % NX).astype(np.float64) / NX
    _CONST["CN"] = (np.cos(ang) / np.sqrt(NX)).astype(np.float32).astype(bf)
    _CONST["SN"] = (-np.sin(ang) / np.sqrt(NX)).astype(np.float32).astype(bf)
    m = np.arange(256)
    ang2 = 2.0 * np.pi * ((m[:, None] * m[None, :]) % 256).astype(np.float64) / 256
    _CONST["C2"] = (np.cos(ang2) / 16.0).astype(np.float32).astype(bf)
    _CONST["S2"] = (-np.sin(ang2) / 16.0).astype(np.float32).astype(bf)
    c = np.arange(64)
    angc = 2.0 * np.pi * ((c[:, None] * c[None, :]) % 64).astype(np.float64) / 64
    bc = np.zeros((256, 256), np.float32)
    bs = np.zeros((256, 256), np.float32)
    for g in range(4):
        bc[g * 64:(g + 1) * 64, g * 64:(g + 1) * 64] = np.cos(angc) / 8.0
        bs[g * 64:(g + 1) * 64, g * 64:(g + 1) * 64] = np.sin(angc) / 8.0
    _CONST["BCS"] = np.concatenate([bc, bs], axis=1)
    _CONST["ident"] = np.eye(128, dtype=np.float32)
    _CONST["Uc"] = np.triu(np.ones((128, 128), np.float32), k=1)
    _CONST["tau"] = np.ascontiguousarray(np.broadcast_to(np.arange(NTMAX, dtype=np.float32)[None, :], (128, NTMAX)))
    _CONST["pconst"] = np.stack([np.arange(128, dtype=np.float32), np.arange(128, dtype=np.float32) + 4096.0], axis=1)
    ro = np.zeros((5, 128, 5, 128), np.int64)
    co = np.zeros((5, 128, 5, 128), np.int64)
    va = np.zeros((5, 128, 5, 128), bool)
    kk = np.arange(128)
    kr, kcol = kk // 64, kk % 64
    qr, qc = kk // 64, kk % 64
    for cl, j in enumerate((0, 1, 2, 14, 15)):
        r = 2 * j + qr
        rstart = np.clip(r - 4, 0, 24)
        cstart = np.clip(qc - 8, 0, 48)
        for i in range(5):
            krow = 2 * (cs_of(j) + i) + kr
            okr = (krow[:, None] >= rstart[None, :]) & (krow[:, None] < rstart[None, :] + 8)
            okc = (kcol[:, None] >= cstart[None, :]) & (kcol[:, None] < cstart[None, :] + 16)
            va[cl, :, i, :] = okr & okc
            ro[cl, :, i, :] = np.clip(krow[:, None] - r[None, :] + 7, 0, 14)
            co[cl, :, i, :] = np.clip(kcol[:, None] - qc[None, :] + 15, 0, 30)
    _CONST["ro"], _CONST["co"], _CONST["va"] = ro, co, va
    return _CONST


def _fm(v):
    v = np.asarray(v, np.float32)
    return np.ascontiguousarray(v.reshape(-1, 128).T)


def prepare_inputs(x, c, ctx, c_ctx, w_ada, b_ada, norm1, norm2, w_in, w_fourier, w_conv, rpb, w_out,
                   w_rg, b_rg, w_re, b_re, w_gate, w_up, w_down, norm_final):
    K = _constants()
    f32 = lambda a: np.ascontiguousarray(np.asarray(a, np.float32))
    shared = {
        "w_ada": f32(w_ada), "w_in": f32(w_in), "w_out": f32(w_out), "wf": f32(w_fourier),
        "CN": K["CN"], "SN": K["SN"], "C2": K["C2"], "S2": K["S2"], "BCS": K["BCS"], "ident": K["ident"],
    }
    shared["bada"] = np.concatenate([_fm(b_ada[0]), _fm(b_ada[1])], axis=1)
    shared["nrm"] = np.concatenate([_fm(norm1[0]), _fm(norm2[0]), _fm(norm1[1]), _fm(norm2[1]), _fm(norm_final)], axis=1)
    wc = np.asarray(w_conv, np.float32)
    shared["wconv"] = np.ascontiguousarray(wc.reshape(2, 3, 2, 128).transpose(3, 0, 2, 1).reshape(128, 12))
    shared["wr"] = np.ascontiguousarray(np.concatenate([np.asarray(w_rg, np.float32), np.asarray(w_re, np.float32)], axis=2))
    brv = np.concatenate([np.asarray(b_rg, np.float32), np.asarray(b_re, np.float32)], axis=1).reshape(1, 72)
    shared["br"] = np.ascontiguousarray(np.broadcast_to(brv, (128, 72)))
    rp = np.asarray(rpb, np.float32)
    g = rp[:, :, K["ro"], K["co"]]
    g = np.where(K["va"][None, None], g, np.float32(-30000.0))
    shared["biasT"] = np.ascontiguousarray(g.transpose(0, 1, 3, 2, 4, 5).reshape(2, 8, 128, 3200).astype(np.float32))
    wgu = np.empty((2, NE, 128, 8192), np.float32)
    wgu[..., 0:4096] = np.asarray(w_gate, np.float32).reshape(2, NE, 8, 128, 512).transpose(0, 1, 3, 2, 4).reshape(2, NE, 128, 4096)
    wgu[..., 4096:8192] = np.asarray(w_up, np.float32).reshape(2, NE, 8, 128, 512).transpose(0, 1, 3, 2, 4).reshape(2, NE, 128, 4096)
    shared["W_gu"] = wgu.reshape(2 * NE * 128, 8192)
    shared["W_dn"] = np.ascontiguousarray(
        np.asarray(w_down, np.float32).reshape(2, NE, 4, 128, 1024).transpose(0, 1, 3, 2, 4).reshape(2 * NE * 128, 4096))
    shared["Uc"] = K["Uc"]
    shared["tau"] = K["tau"]
    shared["pconst"] = K["pconst"]
    in_maps = []
    for b in range(8):
        m = dict(shared)
        m["xin"] = np.ascontiguousarray(np.concatenate([np.asarray(x[b], np.float32), np.asarray(ctx[b], np.float32)], axis=0))
        cvm = np.stack([_fm(c[b]), _fm(c_ctx)], axis=2).reshape(128, 16)
        m["cvec"] = np.ascontiguousarray(cvm)
        in_maps.append(m)
    return in_maps


_PROG = {}


def kernel(**inputs):
    if "nc" not in _PROG:
        _PROG["nc"] = build_program()[0]
    in_maps = prepare_inputs(**inputs)
    res = run_bass_kernel_spmd(_PROG["nc"], in_maps, core_ids=list(range(8)))
    return np.stack([np.asarray(r["out"], np.float32) for r in res.results], axis=0)
```
